# Optimizing a Trainium2 kernel written in Bass

```python
import jax, jax.numpy as jnp
from jax import lax
import numpy as np

D_MODEL = 1024
BATCH = 8
SEQ = 8192
DEPTH = 1

D_RNN = 1280
RNN_BLOCKS = 10
RNN_BW = D_RNN // RNN_BLOCKS
CONV_W = 4
LRU_C = 8.0
N_HEADS = 8
HEAD_DIM = 128
D_ATT = N_HEADS * HEAD_DIM
KV_RANK = 256
IDX_HEADS = 8
IDX_DIM = 64
TOPK_MAX = 256
Q_BLOCK = 128
IDX_SCALE = IDX_DIM ** -0.5 * IDX_HEADS ** -0.5
ATT_SCALE = HEAD_DIM ** -0.5
N_GROUPS = 4
EXP_PER_GROUP = 8
N_EXPERTS = N_GROUPS * EXP_PER_GROUP
TOP_K_INNER = 2
D_EXPERT = 512
MOE_BLOCK = 128
N_BRANCH = 2
EPS = 1e-6

SPLITS = (D_RNN, D_RNN, D_ATT, KV_RANK, IDX_HEADS * IDX_DIM, IDX_DIM, IDX_HEADS, N_BRANCH * D_MODEL)
D_IN = sum(SPLITS)

kernel_name = 'hybrid_rglru_dsa_hmoe_block'


def rmsnorm(x, g):
    xf = x.astype(jnp.float32)
    y = xf * lax.rsqrt(jnp.mean(xf * xf, axis=-1, keepdims=True) + EPS)
    return (y * g.astype(jnp.float32)).astype(x.dtype)


def modulate(x, g, shift, scale):
    return rmsnorm(x, g) * (1 + scale[:, None, :]) + shift[:, None, :]


def causal_dwconv(x, w, b):
    y = lax.conv_general_dilated(x, w[:, None, :], window_strides=(1,), padding=[(CONV_W - 1, 0)],
                                 dimension_numbers=('NWC', 'WIO', 'NWC'), feature_group_count=x.shape[-1])
    return y + b


def rg_lru(x, w_a, b_a, w_x, b_x, lam):
    B_, S_, _ = x.shape
    xb = x.reshape(B_, S_, RNN_BLOCKS, RNN_BW)
    r = jax.nn.sigmoid(jnp.einsum('bsnc,ncd->bsnd', xb, w_a).reshape(B_, S_, D_RNN) + b_a)
    i = jax.nn.sigmoid(jnp.einsum('bsnc,ncd->bsnd', xb, w_x).reshape(B_, S_, D_RNN) + b_x)
    log_a = -LRU_C * jax.nn.softplus(-lam.astype(jnp.float32)) * r.astype(jnp.float32)
    a = jnp.exp(log_a)
    inp = jnp.sqrt(-jnp.expm1(2.0 * log_a)) * (i * x).astype(jnp.float32)

    def combine(lhs, rhs):
        a1, b1 = lhs
        a2, b2 = rhs
        return a1 * a2, a2 * b1 + b2

    _, h = lax.associative_scan(combine, (a, inp), axis=1)
    return h.astype(x.dtype)


def dsa_attention(q, c_kv, q_idx, k_idx, w_idx, w_uk, w_uv):
    B_, S_ = q.shape[0], q.shape[1]
    topk = min(TOPK_MAX, S_ // 4)
    nblk = S_ // Q_BLOCK
    slopes = 2.0 ** (-8.0 * jnp.arange(1, N_HEADS + 1, dtype=jnp.float32) / N_HEADS)
    key_pos = jnp.arange(S_)

    def to_blocks(t):
        return jnp.moveaxis(t.reshape((B_, nblk, Q_BLOCK) + t.shape[2:]), 1, 0)

    def one_block(args):
        qb, qib, wib, q0 = args
        qpos = q0 + jnp.arange(Q_BLOCK)
        causal = key_pos[None, :] <= qpos[:, None]
        rel = jax.nn.relu(jnp.einsum('bqhd,bsd->bqhs', qib, k_idx).astype(jnp.float32))
        score = jnp.einsum('bqh,bqhs->bqs', wib.astype(jnp.float32), rel) * IDX_SCALE
        score = jnp.where(causal[None], score, -jnp.inf)
        _, sel = lax.top_k(score, topk)
        c_sel = jax.vmap(lambda cb, ib: cb[ib])(c_kv, sel)
        q_lat = jnp.einsum('bqhd,rhd->bqhr', qb, w_uk)
        logits = jnp.einsum('bqhr,bqkr->bqhk', q_lat, c_sel).astype(jnp.float32) * ATT_SCALE
        dist = (qpos[None, :, None] - sel).astype(jnp.float32)
        logits = logits - slopes[None, None, :, None] * dist[:, :, None, :]
        valid = sel <= qpos[None, :, None]
        logits = jnp.where(valid[:, :, None, :], logits, -jnp.inf)
        p = jax.nn.softmax(logits, axis=-1).astype(c_sel.dtype)
        o_lat = jnp.einsum('bqhk,bqkr->bqhr', p, c_sel)
        return jnp.einsum('bqhr,rhd->bqhd', o_lat, w_uv)

    out = lax.map(one_block, (to_blocks(q), to_blocks(q_idx), to_blocks(w_idx), jnp.arange(nblk) * Q_BLOCK))
    return jnp.moveaxis(out, 0, 1).reshape(B_, S_, N_HEADS * HEAD_DIM)


def hier_moe(x, w_group, b_group, w_expert, b_expert, w1, w3, w2):
    B_, S_, D_ = x.shape
    T = B_ * S_
    xf = x.reshape(T, D_)
    g_logits = (xf @ w_group + b_group).astype(jnp.float32)
    g_sel = jnp.argmax(g_logits, axis=-1)
    g_w = jnp.take_along_axis(jax.nn.softmax(g_logits, axis=-1), g_sel[:, None], axis=-1)
    e_logits = (xf @ w_expert + b_expert).astype(jnp.float32).reshape(T, N_GROUPS, EXP_PER_GROUP)
    e_logits = jnp.take_along_axis(e_logits, g_sel[:, None, None], axis=1)[:, 0]
    e_top, e_loc = lax.top_k(e_logits, TOP_K_INNER)
    gate = (g_w * jax.nn.softmax(e_top, axis=-1)).reshape(-1)
    eid = (g_sel[:, None] * EXP_PER_GROUP + e_loc).reshape(-1)
    tok = jnp.repeat(jnp.arange(T, dtype=jnp.int32), TOP_K_INNER)
    A = T * TOP_K_INNER
    order = jnp.argsort(eid)
    eid_s = eid[order]
    counts = jnp.bincount(eid, length=N_EXPERTS)
    padded = (counts + MOE_BLOCK - 1) // MOE_BLOCK * MOE_BLOCK
    start = jnp.cumsum(counts) - counts
    pstart = jnp.cumsum(padded) - padded
    dest = pstart[eid_s] + jnp.arange(A) - start[eid_s]
    n_rows = A + N_EXPERTS * MOE_BLOCK
    n_blk = n_rows // MOE_BLOCK
    row_tok = jnp.zeros((n_rows,), jnp.int32).at[dest].set(tok[order])
    row_gate = jnp.zeros((n_rows,), x.dtype).at[dest].set(gate[order].astype(x.dtype))
    blk_exp = jnp.minimum(jnp.searchsorted(jnp.cumsum(padded), jnp.arange(n_blk) * MOE_BLOCK, side='right'),
                          N_EXPERTS - 1)
    xs = xf[row_tok].reshape(n_blk, MOE_BLOCK, D_)

    def expert_block(args):
        xb, e = args
        hid = jax.nn.silu(xb @ w1[e]) * (xb @ w3[e])
        return hid @ w2[e]

    ys = lax.map(expert_block, (xs, blk_exp)).reshape(n_rows, D_)
    out = jax.ops.segment_sum(ys * row_gate[:, None], row_tok, num_segments=T)
    return out.reshape(B_, S_, D_)


def _w(k, shape, fan_in, mult=1.0):
    return jax.random.normal(k, shape, jnp.float32) * (mult * fan_in ** -0.5)


def _small(k, shape):
    return 0.01 * jax.random.normal(k, shape, jnp.float32)


def _gain(k, shape):
    return 1.0 + 0.01 * jax.random.normal(k, shape, jnp.float32)


def setup_inputs(seed: int = 0) -> dict:
    key = jax.random.key(seed)
    ks = list(jax.random.split(key, 32))
    L = DEPTH
    u = jax.random.uniform(ks.pop(), (L, D_RNN), jnp.float32, minval=0.9, maxval=0.999)
    a0 = u ** (1.0 / LRU_C)
    lru_lambda = jnp.log(a0) - jnp.log1p(-a0)
    return {
        'x': jax.random.normal(ks.pop(), (BATCH, SEQ, D_MODEL), jnp.float32),
        'c': jax.random.normal(ks.pop(), (BATCH, D_MODEL), jnp.float32),
        'w_mod': _w(ks.pop(), (L, D_MODEL, 6 * D_MODEL), D_MODEL, 0.5),
        'b_mod': _small(ks.pop(), (L, 6 * D_MODEL)),
        'norm1_g': _gain(ks.pop(), (L, D_MODEL)),
        'w_in': _w(ks.pop(), (L, D_MODEL, D_IN), D_MODEL),
        'conv_w': _w(ks.pop(), (L, CONV_W, D_RNN), CONV_W),
        'conv_b': _small(ks.pop(), (L, D_RNN)),
        'w_rg_a': _w(ks.pop(), (L, RNN_BLOCKS, RNN_BW, RNN_BW), RNN_BW),
        'b_rg_a': _small(ks.pop(), (L, D_RNN)),
        'w_rg_x': _w(ks.pop(), (L, RNN_BLOCKS, RNN_BW, RNN_BW), RNN_BW),
        'b_rg_x': _small(ks.pop(), (L, D_RNN)),
        'lru_lambda': lru_lambda,
        'kv_norm_g': _gain(ks.pop(), (L, KV_RANK)),
        'w_uk': _w(ks.pop(), (L, KV_RANK, N_HEADS, HEAD_DIM), KV_RANK),
        'w_uv': _w(ks.pop(), (L, KV_RANK, N_HEADS, HEAD_DIM), KV_RANK),
        'w_rnn_out': _w(ks.pop(), (L, D_RNN, D_MODEL), D_RNN),
        'w_att_out': _w(ks.pop(), (L, D_ATT, D_MODEL), D_ATT),
        'w_out': _w(ks.pop(), (L, D_MODEL, D_MODEL), D_MODEL),
        'norm2_g': _gain(ks.pop(), (L, D_MODEL)),
        'w_group': _w(ks.pop(), (L, D_MODEL, N_GROUPS), D_MODEL),
        'b_group': _small(ks.pop(), (L, N_GROUPS)),
        'w_expert': _w(ks.pop(), (L, D_MODEL, N_EXPERTS), D_MODEL),
        'b_expert': _small(ks.pop(), (L, N_EXPERTS)),
        'moe_w1': _w(ks.pop(), (L, N_EXPERTS, D_MODEL, D_EXPERT), D_MODEL),
        'moe_w3': _w(ks.pop(), (L, N_EXPERTS, D_MODEL, D_EXPERT), D_MODEL),
        'moe_w2': _w(ks.pop(), (L, N_EXPERTS, D_EXPERT, D_MODEL), D_EXPERT),
        'final_g': _gain(ks.pop(), (D_MODEL,)),
    }


def reference(x, c, w_mod, b_mod, norm1_g, w_in, conv_w, conv_b, w_rg_a, b_rg_a, w_rg_x, b_rg_x,
              lru_lambda, kv_norm_g, w_uk, w_uv, w_rnn_out, w_att_out, w_out, norm2_g, w_group, b_group,
              w_expert, b_expert, moe_w1, moe_w3, moe_w2, final_g):
    B_, S_, _ = x.shape
    split_pts = [int(v) for v in np.cumsum(SPLITS)[:-1]]
    h = x
    for l in range(DEPTH):
        mod = jax.nn.silu(c) @ w_mod[l] + b_mod[l]
        sh1, sc1, g1, sh2, sc2, g2 = jnp.split(mod, 6, axis=-1)
        u = modulate(h, norm1_g[l], sh1, sc1)
        proj = u @ w_in[l]
        xr, gr, q, ckv, qi, ki, wi, mg = jnp.split(proj, split_pts, axis=-1)
        hr = rg_lru(causal_dwconv(xr, conv_w[l], conv_b[l]), w_rg_a[l], b_rg_a[l], w_rg_x[l], b_rg_x[l],
                    lru_lambda[l])
        y_a = (jax.nn.gelu(gr) * hr) @ w_rnn_out[l]
        att = dsa_attention(q.reshape(B_, S_, N_HEADS, HEAD_DIM), rmsnorm(ckv, kv_norm_g[l]),
                            qi.reshape(B_, S_, IDX_HEADS, IDX_DIM), ki, wi, w_uk[l], w_uv[l])
        y_b = att @ w_att_out[l]
        ga, gb = jnp.split(jax.nn.sigmoid(mg), N_BRANCH, axis=-1)
        mix = (ga * y_a + gb * y_b) @ w_out[l]
        h = h + g1[:, None, :] * mix
        u2 = modulate(h, norm2_g[l], sh2, sc2)
        h = h + g2[:, None, :] * hier_moe(u2, w_group[l], b_group[l], w_expert[l], b_expert[l],
                                          moe_w1[l], moe_w3[l], moe_w2[l])
    return rmsnorm(h, final_g)
```

```python
import numpy as np
from contextlib import ExitStack
import concourse.bass as bass
import concourse.mybir as mybir
from concourse.bass_utils import run_bass_kernel_spmd

F32 = mybir.dt.float32
BF16 = mybir.dt.bfloat16
AF = mybir.ActivationFunctionType
ALU = mybir.AluOpType
AX = mybir.AxisListType

D = 1024
DR = 1280
NRB = 10
NH = 8
HD = 128
R = 256
IH = 8
IDIM = 64
TOPK = 256
NE = 32
NG = 4
EPG = 8
DE = 512
EPS = 1e-6
IDX_SCALE = IDIM ** -0.5 * IH ** -0.5
ATT_SCALE = HD ** -0.5
NEG = -30000.0
NIT = 16
FM1 = 2560 + 2048
FM2 = 1024 + 512 + 128
TMC = 264


class Sched:
    def __init__(self, nc, n_dma_slots=10):
        self.nc = nc
        self.eng = {"pe": nc.tensor, "act": nc.scalar, "dve": nc.vector, "pool": nc.gpsimd, "sp": nc.sync}
        self.sem, self.cnt, self._cms = {}, {}, []
        for e in self.eng:
            cm = nc.semaphore("prog_" + e)
            self.sem[e] = cm.__enter__()
            self._cms.append(cm)
            self.cnt[e] = 0
        self.dma_q = ("sp", "pool")
        self.dslots = {}
        for q in self.dma_q:
            sl = []
            for i in range(n_dma_slots):
                cm = nc.semaphore(f"dma_{q}_{i}")
                sl.append([cm.__enter__(), 0])
                self._cms.append(cm)
            self.dslots[q] = sl
        self.dnext = {q: 0 for q in self.dma_q}
        self.waited = {e: {} for e in self.eng}
        self.state = {}

    def close(self):
        for cm in reversed(self._cms):
            cm.__exit__(None, None, None)

    @staticmethod
    def _key(t):
        return t if isinstance(t, str) else id(t)

    def _deps(self, reads, writes):
        deps = []
        for t in reads:
            st = self.state.get(self._key(t))
            if st and st[0] is not None:
                deps.append(st[0])
        for t in writes:
            st = self.state.get(self._key(t))
            if st:
                if st[0] is not None:
                    deps.append(st[0])
                deps.extend(st[1])
        return deps

    def _wait(self, e, deps):
        w = self.waited[e]
        best = {}
        for (sname, sem, val) in deps:
            if e == "pe" and sname == "prog_pe":
                continue
            if w.get(sname, 0) >= val:
                continue
            if sname not in best or best[sname][1] < val:
                best[sname] = (sem, val)
        for sname, (sem, val) in best.items():
            self.eng[e].wait_ge(sem, val)
            w[sname] = val

    def _record(self, tok, reads, writes):
        for t in writes:
            self.state[self._key(t)] = [tok, []]
        for t in reads:
            st = self.state.setdefault(self._key(t), [None, []])
            st[1] = [x for x in st[1] if x[0] != tok[0]] + [tok]

    def op(self, e, fn, reads=(), writes=()):
        self._wait(e, self._deps(reads, writes))
        ins = fn()
        self.cnt[e] += 1
        ins.then_inc(self.sem[e], 1)
        tok = ("prog_" + e, self.sem[e], self.cnt[e])
        self._record(tok, reads, writes)
        return tok

    def dma(self, q, out, in_, reads=(), writes=()):
        sl = self.dslots[q]
        i = self.dnext[q]
        self.dnext[q] = (i + 1) % len(sl)
        sem, used = sl[i]
        sname = f"dma_{q}_{i}"
        deps = self._deps(reads, writes)
        if used:
            deps.append((sname, sem, used))
        self._wait(q, deps)
        self.eng[q].dma_start(out=out, in_=in_).then_inc(sem, 16)
        sl[i][1] = used + 16
        tok = (sname, sem, used + 16)
        self._record(tok, reads, writes)
        return tok

    def barrier(self):
        toks = [("prog_" + e, self.sem[e], self.cnt[e]) for e in self.eng if self.cnt[e]]
        for q in self.dma_q:
            for i, (sem, used) in enumerate(self.dslots[q]):
                if used:
                    toks.append((f"dma_{q}_{i}", sem, used))
        for e in self.eng:
            self._wait(e, toks)
        self.state = {}


def build(S, debug=False):
    NB = S // 128
    NT = S // 512
    TC = min(1024, S)
    NTC = S // TC
    nc = bass.Bass("TRN2", target_bir_lowering=False)

    def din(name, shape, dt=F32):
        return nc.dram_tensor(name, list(shape), dt, kind="ExternalInput").ap()

    def dscr(name, shape, dt):
        return nc.dram_tensor(name, list(shape), dt, kind="ExternalOutput" if debug else "Internal").ap()

    x = din("x", [S, D])
    c_col = din("c_col", [128, 8])
    w_mod = din("w_mod", [128, 8, 6 * D])
    b_mod_col = din("b_mod_col", [128, 48])
    b_mod_rep = din("b_mod_rep", [128, 6 * D])
    n1_col = din("n1_col", [128, 8])
    n2_col = din("n2_col", [128, 8])
    w_fm1 = din("w_fm1", [128, 8, FM1])
    w_fm2 = din("w_fm2", [128, 8, FM2])
    w_tm = din("w_tm", [128, 8, TMC])
    conv_w = din("conv_w", [128, NRB, 4])
    conv_b = din("conv_b", [128, NRB])
    w_rg_a = din("w_rg_a", [128, NRB, 128])
    w_rg_x = din("w_rg_x", [128, NRB, 128])
    b_rg_a = din("b_rg_a", [128, NRB])
    b_rg_x = din("b_rg_x", [128, NRB])
    lam = din("lam", [128, NRB])
    gkv_rep = din("gkv_rep", [128, R])
    w_ukT = din("w_ukT", [128, NH, R])
    w_uv = din("w_uv", [128, 2, NH * HD])
    w_rnn = din("w_rnn", [128, NRB, D])
    w_att = din("w_att", [128, 8, D])
    w_o = din("w_o", [128, 8, D])
    w_ge = din("w_ge", [128, 8, 36])
    b_ge_rep = din("b_ge_rep", [128, 36])
    moe_w1 = din("moe_w1", [NE, D, DE])
    moe_w3 = din("moe_w3", [NE, D, DE])
    moe_w2 = din("moe_w2", [NE, DE, D])
    fg_rep = din("fg_rep", [128, D])
    out = nc.dram_tensor("out", [S, D], F32, kind="ExternalOutput").ap()

    u1_s = dscr("u1_s", [128, 8, S], BF16)
    qlat_s = dscr("qlat_s", [128, 2, NH, S], BF16)
    qi_s = dscr("qi_s", [128, 4, S], BF16)
    sg_s = dscr("sg_s", [128, 16, S], BF16)
    ya_s = dscr("ya_s", [128, 8, S], BF16)
    att_s = dscr("att_s", [128, 8, S], BF16)
    h_s = dscr("h_s", [S, D], F32)
    u2_s = dscr("u2_s", [128, 8, S], BF16)
    dbg = {}
    if debug:
        dbg["score"] = nc.dram_tensor("dbg_score", [NB, 128, S], F32, kind="ExternalOutput").ap()
        dbg["thr"] = nc.dram_tensor("dbg_thr", [128, NB, 4], F32, kind="ExternalOutput").ap()
        dbg["wt"] = nc.dram_tensor("dbg_wt", [128, NB, NE], F32, kind="ExternalOutput").ap()

    SC = Sched(nc)
    op = SC.op
    V, A, P, T = nc.vector, nc.scalar, nc.gpsimd, nc.tensor

    def MM(o, terms, reads, writes):
        def f():
            n = len(terms)
            ins = None
            for i, (l, r) in enumerate(terms):
                ins = T.matmul(o, lhsT=l, rhs=r, start=(i == 0), stop=(i == n - 1))
            return ins
        return op("pe", f, reads, writes)

    def wload(dst, src, q="pool"):
        n = dst.shape[1]
        for i in range(n):
            SC.dma(q, dst[:, i], src[:, i], writes=[dst])

    uid = [0]

    def sbt(es, name, shape, dt):
        uid[0] += 1
        return es.enter_context(nc.sbuf_tensor(f"{name}_{uid[0]}", list(shape), dt))

    def pst(es, name, shape, dt=F32):
        uid[0] += 1
        return es.enter_context(nc.psum_tensor(f"{name}_{uid[0]}", list(shape), dt))

    def mod_rows(grow, g0, pr):
        with ExitStack() as es:
            cc = sbt(es, "ccr", [128, 8], F32)
            scl = sbt(es, "sclr", [128, 8], F32)
            screp = sbt(es, "screp", [128, 8, 128], F32)
            wm = sbt(es, "wmr", [128, 8, 512], F32)
            brow = sbt(es, "brow", [128, 512], F32)
            SC.dma("sp", cc[:], c_col, writes=[cc])
            op("act", lambda: A.activation(out=scl[:], in_=cc[:], func=AF.Silu), reads=[cc], writes=[scl])
            for kc in range(8):
                op("dve", lambda kc=kc: V.tensor_copy(out=screp[:, kc, :], in_=scl[:, kc:kc + 1].to_broadcast([128, 128])),
                   reads=[scl], writes=[screp])
            for half in range(2):
                g = g0 + half
                SC.dma("sp", wm[:], w_mod[:, :, g * 512:(g + 1) * 512], writes=[wm])
                SC.dma("sp", brow[:], b_mod_rep[:, g * 512:(g + 1) * 512], writes=[brow])
                MM(pr[:], [(screp[:, kc, :], wm[:, kc, :]) for kc in range(8)], [screp, wm], [pr])
                op("dve", lambda half=half: V.tensor_tensor(out=grow[:, half * 512:(half + 1) * 512], in0=pr[:],
                                                            in1=brow[:], op=ALU.add), reads=[pr, brow], writes=[grow])
            SC.barrier()

    with ExitStack() as es0:
        eps_c = sbt(es0, "eps_c", [128, 1], F32)
        one_c = sbt(es0, "one_c", [128, 1], F32)
        ident_f = sbt(es0, "ident_f", [128, 128], F32)
        ident_b = sbt(es0, "ident_b", [128, 128], BF16)
        ones_b = sbt(es0, "ones_b", [128, 128], BF16)
        irep = sbt(es0, "irep", [128, 4, 128], BF16)
        caus = sbt(es0, "caus", [128, 128], F32)
        cpos = sbt(es0, "cpos", [128, 128], F32)
        gpos = sbt(es0, "gpos", [128, 128], F32)
        nsl = sbt(es0, "nsl", [128, NH], F32)
        A1 = sbt(es0, "A1", [128, 8], F32)
        B1 = sbt(es0, "B1", [128, 8], F32)
        A2 = sbt(es0, "A2", [128, 8], F32)
        B2 = sbt(es0, "B2", [128, 8], F32)

        op("pool", lambda: P.memset(eps_c[:], EPS), writes=[eps_c])
        op("pool", lambda: P.memset(one_c[:], 1.0), writes=[one_c])
        op("pool", lambda: P.memset(ident_f[:], 1.0), writes=[ident_f])
        op("pool", lambda: P.affine_select(out=ident_f[:], in_=ident_f[:], pattern=[[-1, 128]],
                                           compare_op=ALU.is_equal, fill=0.0, base=0, channel_multiplier=1),
           reads=[ident_f], writes=[ident_f])
        op("dve", lambda: V.tensor_copy(out=ident_b[:], in_=ident_f[:]), reads=[ident_f], writes=[ident_b])
        op("pool", lambda: P.memset(ones_b[:], 1.0), writes=[ones_b])
        for r_ in range(4):
            op("dve", lambda r_=r_: V.tensor_copy(out=irep[:, r_, :], in_=ident_f[:]), reads=[ident_f], writes=[irep])
        op("pool", lambda: P.memset(caus[:], 0.0), writes=[caus])
        op("pool", lambda: P.affine_select(out=caus[:], in_=caus[:], pattern=[[-1, 128]],
                                           compare_op=ALU.is_ge, fill=NEG, base=0, channel_multiplier=1),
           reads=[caus], writes=[caus])
        op("dve", lambda: V.tensor_scalar(out=cpos[:], in0=caus[:], scalar1=-1.0, scalar2=None, op0=ALU.mult),
           reads=[caus], writes=[cpos])
        op("pool", lambda: P.iota(gpos[:], pattern=[[64, 128]], base=63, channel_multiplier=0,
                                  allow_small_or_imprecise_dtypes=True), writes=[gpos])
        for h in range(NH):
            op("pool", lambda h=h: P.memset(nsl[:, h:h + 1], -(2.0 ** -(h + 1))), writes=[nsl])

        with ExitStack() as es:
            cc = sbt(es, "cc", [128, 8], F32)
            scl = sbt(es, "scl", [128, 8], F32)
            bmc = sbt(es, "bmc", [128, 48], F32)
            modT = sbt(es, "modT", [128, 48], F32)
            n1c = sbt(es, "n1c", [128, 8], F32)
            n2c = sbt(es, "n2c", [128, 8], F32)
            wm = [sbt(es, f"wm{i}", [128, 8, 512], F32) for i in range(2)]
            pm = pst(es, "pm", [128, 48])
            SC.dma("sp", cc[:], c_col, writes=[cc])
            SC.dma("sp", bmc[:], b_mod_col, writes=[bmc])
            SC.dma("sp", n1c[:], n1_col, writes=[n1c])
            SC.dma("sp", n2c[:], n2_col, writes=[n2c])
            op("act", lambda: A.activation(out=scl[:], in_=cc[:], func=AF.Silu), reads=[cc], writes=[scl])
            for g in range(12):
                wb = wm[g % 2]
                SC.dma("sp", wb[:], w_mod[:, :, g * 512:(g + 1) * 512], writes=[wb])
                def f(g=g, wb=wb):
                    ins = None
                    for j in range(4):
                        for kc in range(8):
                            ins = T.matmul(pm[:, 4 * g + j:4 * g + j + 1], lhsT=wb[:, kc, j * 128:(j + 1) * 128],
                                           rhs=scl[:, kc:kc + 1], start=(kc == 0), stop=(kc == 7))
                    return ins
                op("pe", f, reads=[wb, scl], writes=[pm])
            op("dve", lambda: V.tensor_tensor(out=modT[:], in0=pm[:], in1=bmc[:], op=ALU.add),
               reads=[pm, bmc], writes=[modT])
            op("dve", lambda: V.scalar_tensor_tensor(out=A1[:], in0=modT[:, 8:16], scalar=1.0, in1=n1c[:],
                                                     op0=ALU.add, op1=ALU.mult), reads=[modT, n1c], writes=[A1])
            op("dve", lambda: V.tensor_copy(out=B1[:], in_=modT[:, 0:8]), reads=[modT], writes=[B1])
            op("dve", lambda: V.scalar_tensor_tensor(out=A2[:], in0=modT[:, 32:40], scalar=1.0, in1=n2c[:],
                                                     op0=ALU.add, op1=ALU.mult), reads=[modT, n2c], writes=[A2])
            op("dve", lambda: V.tensor_copy(out=B2[:], in_=modT[:, 24:32]), reads=[modT], writes=[B2])
            SC.barrier()

        with ExitStack() as es:
            wf = sbt(es, "wf1", [128, 8, FM1], BF16)
            wrn = sbt(es, "wrn", [128, NRB, D], BF16)
            wa = sbt(es, "wa", [128, NRB, 128], BF16)
            wx = sbt(es, "wx", [128, NRB, 128], BF16)
            cw = sbt(es, "cw", [128, NRB, 4], F32)
            cb = sbt(es, "cb", [128, NRB], F32)
            ba = sbt(es, "ba", [128, NRB], F32)
            bx = sbt(es, "bx", [128, NRB], F32)
            lm = sbt(es, "lm", [128, NRB], F32)
            cL = sbt(es, "cL", [128, NRB], F32)
            cL2 = sbt(es, "cL2", [128, NRB], F32)
            hprev = sbt(es, "hprev", [128, NRB], F32)
            xr_buf = sbt(es, "xr_buf", [128, NRB, 515], F32)
            xs = [sbt(es, f"xs{i}", [128, D], F32) for i in range(2)]
            xn = [sbt(es, f"xn{i}", [128, D], BF16) for i in range(2)]
            junk = sbt(es, "junkA", [128, D], BF16)
            ssq = [sbt(es, f"ssq{i}", [128, 1], F32) for i in range(2)]
            rs = [sbt(es, f"rs{i}", [128, 1], F32) for i in range(2)]
            uT = sbt(es, "uT", [128, 8, 512], BF16)
            gg = sbt(es, "gg", [128, NRB, 512], BF16)
            hg = gg
            sgb = [sbt(es, f"sgb{i}", [128, 512], BF16) for i in range(2)] * 2
            yab = [sbt(es, f"yab{i}", [128, 512], BF16) for i in range(2)]
            x2 = [sbt(es, f"x2_{i}", [128, 512], F32) for i in range(1)] * 2
            tt = [sbt(es, f"tt_{i}", [128, 512], F32) for i in range(1)] * 2
            xc = [sbt(es, f"xc{i}", [128, 512], F32) for i in range(2)]
            xcb = [sbt(es, f"xcb{i}", [128, 512], BF16) for i in range(2)]
            rr = [sbt(es, f"rr{i}", [128, 512], F32) for i in range(2)]
            ig = [sbt(es, f"ig{i}", [128, 512], F32) for i in range(2)]
            aa = [sbt(es, f"aa{i}", [128, 512], F32) for i in range(2)]
            a2 = [sbt(es, f"a2{i}", [128, 512], F32) for i in range(2)]
            bb = [sbt(es, f"bb{i}", [128, 512], F32) for i in range(2)]
            hs = [sbt(es, f"hs{i}", [128, 512], F32) for i in range(2)]
            ptb = pst(es, "ptbA", [128, 8, 128], BF16)
            pf = [pst(es, f"pfA{i}", [128, 512]) for i in range(2)]
            prga = [pst(es, f"prgaA{i}", [128, 512]) for i in range(2)]
            prgx = [pst(es, f"prgxA{i}", [128, 512]) for i in range(2)]
            pya = [pst(es, f"pyaA{i}", [128, 512]) for i in range(1)] * 2

            wload(wf, w_fm1)
            wload(wrn, w_rnn)
            SC.dma("pool", wa[:], w_rg_a, writes=[wa])
            SC.dma("pool", wx[:], w_rg_x, writes=[wx])
            for (dst, src) in ((cw, conv_w), (cb, conv_b), (ba, b_rg_a), (bx, b_rg_x), (lm, lam)):
                SC.dma("sp", dst[:], src, writes=[dst])
            op("act", lambda: A.activation(out=cL[:], in_=lm[:], func=AF.Exp, scale=-1.0), reads=[lm], writes=[cL])
            op("act", lambda: A.activation(out=cL[:], in_=cL[:], func=AF.Ln, bias=one_c[:], scale=1.0),
               reads=[cL, one_c], writes=[cL])
            op("dve", lambda: V.tensor_scalar(out=cL2[:], in0=cL[:], scalar1=-16.0, scalar2=None, op0=ALU.mult),
               reads=[cL], writes=[cL2])
            op("dve", lambda: V.tensor_scalar(out=cL[:], in0=cL[:], scalar1=-8.0, scalar2=None, op0=ALU.mult),
               reads=[cL], writes=[cL])
            op("pool", lambda: P.memset(hprev[:], 0.0), writes=[hprev])
            op("pool", lambda: P.memset(xr_buf[:], 0.0), writes=[xr_buf])

            for t in range(NT):
                t0 = t * 512
                for s in range(4):
                    b = s % 2
                    SC.dma("sp", xs[b][:], x[t0 + s * 128:t0 + (s + 1) * 128, :], writes=[xs[b]])
                    op("act", lambda b=b: A.activation(out=junk[:], in_=xs[b][:], func=AF.Square, accum_out=ssq[b][:]),
                       reads=[xs[b]], writes=[junk, ssq[b]])
                    op("act", lambda b=b: A.activation(out=rs[b][:], in_=ssq[b][:], func=AF.Sqrt, bias=eps_c[:],
                                                       scale=1.0 / D), reads=[ssq[b], eps_c], writes=[rs[b]])
                    op("dve", lambda b=b: V.reciprocal(out=rs[b][:], in_=rs[b][:]), reads=[rs[b]], writes=[rs[b]])
                    op("dve", lambda b=b: V.tensor_scalar(out=xn[b][:], in0=xs[b][:], scalar1=rs[b][:], scalar2=None,
                                                          op0=ALU.mult), reads=[xs[b], rs[b]], writes=[xn[b]])
                    def tr(b=b):
                        ins = None
                        for c in range(8):
                            ins = T.transpose(out=ptb[:, c, :], in_=xn[b][:, c * 128:(c + 1) * 128], identity=ident_b[:])
                        return ins
                    op("pe", tr, reads=[xn[b], ident_b], writes=[ptb])
                    for c in range(8):
                        if c % 2 == 0:
                            op("dve", lambda c=c, s=s: V.tensor_scalar(
                                out=uT[:, c, s * 128:(s + 1) * 128], in0=ptb[:, c, :], scalar1=A1[:, c:c + 1],
                                scalar2=B1[:, c:c + 1], op0=ALU.mult, op1=ALU.add), reads=[ptb, A1, B1], writes=[uT])
                        else:
                            op("act", lambda c=c, s=s: A.activation(
                                out=uT[:, c, s * 128:(s + 1) * 128], in_=ptb[:, c, :], func=AF.Identity,
                                bias=B1[:, c:c + 1], scale=A1[:, c:c + 1]), reads=[ptb, A1, B1], writes=[uT])
                SC.dma("sp", u1_s[:, :, t0:t0 + 512], uT[:], reads=[uT])
                op("pool", lambda: P.tensor_copy(out=xr_buf[:, :, 0:3], in_=xr_buf[:, :, 512:515]),
                   reads=[xr_buf], writes=[xr_buf])
                for j in range(FM1 // 128):
                    pp = pf[j % 2]
                    MM(pp[:], [(wf[:, kc, j * 128:(j + 1) * 128], uT[:, kc, :]) for kc in range(8)], [wf, uT], [pp])
                    if j < 10:
                        op("act", lambda pp=pp, j=j: A.copy(out=xr_buf[:, j, 3:515], in_=pp[:]),
                           reads=[pp], writes=[xr_buf])
                    elif j < 20:
                        n = j - 10
                        b = n % 2
                        op("act", lambda pp=pp, b=b: A.activation(out=x2[b][:], in_=pp[:], func=AF.Square),
                           reads=[pp], writes=[x2[b]])
                        op("dve", lambda b=b: V.tensor_scalar(out=x2[b][:], in0=x2[b][:], scalar1=0.044715, scalar2=1.0,
                                                              op0=ALU.mult, op1=ALU.add), reads=[x2[b]], writes=[x2[b]])
                        op("dve", lambda pp=pp, b=b: V.tensor_tensor(out=x2[b][:], in0=x2[b][:], in1=pp[:], op=ALU.mult),
                           reads=[x2[b], pp], writes=[x2[b]])
                        op("act", lambda b=b: A.activation(out=tt[b][:], in_=x2[b][:], func=AF.Sigmoid,
                                                           scale=1.5957691216057308), reads=[x2[b]], writes=[tt[b]])
                        op("dve", lambda pp=pp, b=b, n=n: V.tensor_tensor(out=gg[:, n, :], in0=tt[b][:], in1=pp[:],
                                                                          op=ALU.mult), reads=[tt[b], pp], writes=[gg])
                    else:
                        m = j - 20
                        sb_ = sgb[m % 4]
                        op("act", lambda pp=pp, sb_=sb_: A.activation(out=sb_[:], in_=pp[:], func=AF.Sigmoid),
                           reads=[pp], writes=[sb_])
                        SC.dma("sp", sg_s[:, m, t0:t0 + 512], sb_[:], reads=[sb_])
                def stage1(n):
                    b = n % 2
                    op("dve", lambda n=n, b=b: V.tensor_scalar(out=xc[b][:], in0=xr_buf[:, n, 0:512],
                                                               scalar1=cw[:, n, 0:1], scalar2=cb[:, n:n + 1],
                                                               op0=ALU.mult, op1=ALU.add),
                       reads=[xr_buf, cw, cb], writes=[xc[b]])
                    for k in range(1, 4):
                        op("dve", lambda n=n, b=b, k=k: V.scalar_tensor_tensor(
                            out=xc[b][:], in0=xr_buf[:, n, k:k + 512], scalar=cw[:, n, k:k + 1], in1=xc[b][:],
                            op0=ALU.mult, op1=ALU.add), reads=[xr_buf, cw, xc[b]], writes=[xc[b]])
                    op("act", lambda b=b: A.copy(out=xcb[b][:], in_=xc[b][:]), reads=[xc[b]], writes=[xcb[b]])
                    MM(prga[b][:], [(wa[:, n, :], xcb[b][:])], [wa, xcb[b]], [prga[b]])
                    MM(prgx[b][:], [(wx[:, n, :], xcb[b][:])], [wx, xcb[b]], [prgx[b]])

                def stage2(n):
                    b = n % 2
                    op("act", lambda n=n, b=b: A.activation(out=rr[b][:], in_=prga[b][:], func=AF.Sigmoid,
                                                            bias=ba[:, n:n + 1], scale=1.0),
                       reads=[prga[b], ba], writes=[rr[b]])
                    op("act", lambda n=n, b=b: A.activation(out=ig[b][:], in_=prgx[b][:], func=AF.Sigmoid,
                                                            bias=bx[:, n:n + 1], scale=1.0),
                       reads=[prgx[b], bx], writes=[ig[b]])
                    op("act", lambda n=n, b=b: A.activation(out=aa[b][:], in_=rr[b][:], func=AF.Exp,
                                                            scale=cL[:, n:n + 1]), reads=[rr[b], cL], writes=[aa[b]])
                    op("act", lambda n=n, b=b: A.activation(out=a2[b][:], in_=rr[b][:], func=AF.Exp,
                                                            scale=cL2[:, n:n + 1]), reads=[rr[b], cL2], writes=[a2[b]])
                    op("act", lambda b=b: A.activation(out=a2[b][:], in_=a2[b][:], func=AF.Sqrt, bias=one_c[:],
                                                       scale=-1.0), reads=[a2[b], one_c], writes=[a2[b]])
                    op("pool", lambda b=b: P.tensor_tensor(out=bb[b][:], in0=ig[b][:], in1=xc[b][:], op=ALU.mult),
                       reads=[ig[b], xc[b]], writes=[bb[b]])
                    op("dve", lambda b=b: V.tensor_tensor(out=bb[b][:], in0=bb[b][:], in1=a2[b][:], op=ALU.mult),
                       reads=[bb[b], a2[b]], writes=[bb[b]])
                    op("dve", lambda n=n, b=b: V.tensor_tensor_scan(out=hs[b][:], data0=aa[b][:], data1=bb[b][:],
                                                                    initial=hprev[:, n:n + 1], op0=ALU.mult,
                                                                    op1=ALU.add),
                       reads=[aa[b], bb[b], hprev], writes=[hs[b]])
                    op("dve", lambda n=n, b=b: V.tensor_copy(out=hprev[:, n:n + 1], in_=hs[b][:, 511:512]),
                       reads=[hs[b]], writes=[hprev])
                    op("pool", lambda n=n, b=b: P.tensor_tensor(out=hg[:, n, :], in0=hs[b][:], in1=gg[:, n, :],
                                                                op=ALU.mult), reads=[hs[b], gg], writes=[hg])

                stage1(0)
                for n in range(NRB):
                    if n + 1 < NRB:
                        stage1(n + 1)
                    stage2(n)
                pyas = [pya[0], pf[0], pf[1]]
                for dc in range(8):
                    pp = pyas[dc % 3]
                    MM(pp[:], [(wrn[:, n, dc * 128:(dc + 1) * 128], hg[:, n, :]) for n in range(NRB)], [wrn, hg], [pp])
                    yb_ = yab[dc % 2]
                    if dc % 2 == 0:
                        op("act", lambda pp=pp, yb_=yb_: A.copy(out=yb_[:], in_=pp[:]), reads=[pp], writes=[yb_])
                    else:
                        op("dve", lambda pp=pp, yb_=yb_: V.tensor_copy(out=yb_[:], in_=pp[:]), reads=[pp], writes=[yb_])
                    SC.dma("sp", ya_s[:, dc, t0:t0 + 512], yb_[:], reads=[yb_])
            SC.barrier()

        with ExitStack() as esm:
            kiT2 = sbt(esm, "kiT2", [128, S], BF16)
            Cc = sbt(esm, "Cc", [128, NB, R], BF16)
            CT = sbt(esm, "CT", [128, 2, S], BF16)
            WI = sbt(esm, "WI", [128, NB, 8], F32)

            with ExitStack() as es:
                wf = sbt(es, "wf2", [128, 8, FM2], BF16)
                wt = sbt(es, "wtm", [128, 8, TMC], BF16)
                wuk = sbt(es, "wuk", [128, NH, R], BF16)
                gkv = sbt(es, "gkv", [128, R], F32)
                uT2 = [sbt(es, f"uT2_{i}", [128, 8, 512], BF16) for i in range(2)]
                qT = sbt(es, "qT", [128, 8, 512], BF16)
                qlb = [sbt(es, f"qlb{i}", [128, 2, 512], BF16) for i in range(2)]
                qib = sbt(es, "qib", [128, 4, 512], BF16)
                junk = sbt(es, "junkA2", [128, R], F32)
                ssq = [sbt(es, f"ssqc{i}", [128, 1], F32) for i in range(2)]
                rs = [sbt(es, f"rsc{i}", [128, 1], F32) for i in range(2)]
                pf = [pst(es, f"pfB{i}", [128, 512]) for i in range(3)]
                ptm = [pst(es, f"ptm{i}", [128, 512]) for i in range(2)]
                ptb = pst(es, "ptbB", [128, 2, 128], BF16)
                pql = [pst(es, f"pql{i}", [128, 512]) for i in range(2)]
                wload(wf, w_fm2)
                SC.dma("pool", wt[:], w_tm, writes=[wt])
                SC.dma("pool", wuk[:], w_ukT, writes=[wuk])
                SC.dma("sp", gkv[:], gkv_rep, writes=[gkv])
                for t in range(NT):
                    t0 = t * 512
                    u = uT2[t % 2]
                    SC.dma("sp", u[:], u1_s[:, :, t0:t0 + 512], writes=[u])
                    for j in range(FM2 // 128):
                        pp = pf[j % 3]
                        MM(pp[:], [(wf[:, kc, j * 128:(j + 1) * 128], u[:, kc, :]) for kc in range(8)], [wf, u], [pp])
                        if j < 8:
                            e_ = "act" if j % 2 else "dve"
                            if e_ == "act":
                                op("act", lambda pp=pp, j=j: A.copy(out=qT[:, j, :], in_=pp[:]), reads=[pp], writes=[qT])
                            else:
                                op("dve", lambda pp=pp, j=j: V.tensor_copy(out=qT[:, j, :], in_=pp[:]),
                                   reads=[pp], writes=[qT])
                        elif j < 12:
                            op("act", lambda pp=pp, j=j: A.copy(out=qib[:, j - 8, :], in_=pp[:]), reads=[pp], writes=[qib])
                        else:
                            op("dve", lambda pp=pp: V.tensor_copy(out=kiT2[:, t0:t0 + 512], in_=pp[:]),
                               reads=[pp], writes=[kiT2])
                    SC.dma("sp", qi_s[:, :, t0:t0 + 512], qib[:], reads=[qib])
                    for h in range(NH):
                        ql = qlb[h % 2]
                        for rc in range(2):
                            pp = pql[rc]
                            MM(pp[:], [(wuk[:, h, rc * 128:(rc + 1) * 128], qT[:, h, :])], [wuk, qT], [pp])
                            if rc == 0:
                                op("act", lambda pp=pp, ql=ql: A.mul(out=ql[:, 0, :], in_=pp[:], mul=ATT_SCALE),
                                   reads=[pp], writes=[ql])
                            else:
                                op("dve", lambda pp=pp, ql=ql: V.tensor_scalar(out=ql[:, 1, :], in0=pp[:],
                                                                               scalar1=ATT_SCALE, scalar2=None,
                                                                               op0=ALU.mult), reads=[pp], writes=[ql])
                        SC.dma("sp", qlat_s[:, :, h, t0:t0 + 512], ql[:], reads=[ql])
                    for s in range(4):
                        blk = t * 4 + s
                        b = s % 2
                        pp = ptm[b]
                        MM(pp[:, 0:TMC], [(u[:, kc, s * 128:(s + 1) * 128], wt[:, kc, :]) for kc in range(8)],
                           [u, wt], [pp])
                        op("act", lambda pp=pp, b=b: A.activation(out=junk[:], in_=pp[:, 0:R], func=AF.Square,
                                                                  accum_out=ssq[b][:]), reads=[pp], writes=[junk, ssq[b]])
                        op("act", lambda b=b: A.activation(out=rs[b][:], in_=ssq[b][:], func=AF.Sqrt, bias=eps_c[:],
                                                           scale=1.0 / R), reads=[ssq[b], eps_c], writes=[rs[b]])
                        op("dve", lambda b=b: V.reciprocal(out=rs[b][:], in_=rs[b][:]), reads=[rs[b]], writes=[rs[b]])
                        op("dve", lambda pp=pp, b=b, blk=blk: V.scalar_tensor_tensor(
                            out=Cc[:, blk, :], in0=pp[:, 0:R], scalar=rs[b][:], in1=gkv[:], op0=ALU.mult, op1=ALU.mult),
                           reads=[pp, rs[b], gkv], writes=[Cc])
                        op("act", lambda pp=pp, blk=blk: A.copy(out=WI[:, blk, :], in_=pp[:, R:R + 8]),
                           reads=[pp], writes=[WI])
                        def tr(blk=blk):
                            ins = None
                            for rc in range(2):
                                ins = T.transpose(out=ptb[:, rc, :], in_=Cc[:, blk, rc * 128:(rc + 1) * 128],
                                                  identity=ident_b[:])
                            return ins
                        op("pe", tr, reads=[Cc, ident_b], writes=[ptb])
                        op("act", lambda blk=blk: A.copy(out=CT[:, :, blk * 128:(blk + 1) * 128], in_=ptb[:]),
                           reads=[ptb], writes=[CT])
                SC.barrier()

            with ExitStack() as es:
                LPOS = sbt(es, "LPOS", [128, NB, 128], BF16)
                RB = [sbt(es, f"RB{i}", [128, 2, 512], BF16) for i in range(2)]
                wuv = sbt(es, "wuv", [128, 2, NH * HD], BF16)
                score = sbt(es, "score", [128, S], F32)
                MB = [sbt(es, f"MB{i}", [128, S], BF16) for i in range(2)]
                qiblk = [sbt(es, f"qiblk{i}", [128, 4, 128], BF16) for i in range(2)]
                qlblk = [sbt(es, f"qlblk{i}", [128, 2, NH, 128], BF16) for i in range(1)] * 2
                Dh = [sbt(es, f"Dh{i}", [128, IH, 128], BF16) for i in range(1)] * 2
                Rh = [sbt(es, f"Rh{i}", [128, 512], BF16) for i in range(4)]
                qz = [sbt(es, f"qz{i}", [128, IH, 128], BF16) for i in range(2)]
                PT = [sbt(es, f"PT{i}", [128, 512], BF16) for i in range(3)]
                m8 = sbt(es, "m8", [128, 8], F32)
                rmin = sbt(es, "rmin", [128, 2], F32)
                tmpd = sbt(es, "tmpd", [128, 128], F32)
                lo = sbt(es, "lo", [128, 1], F32)
                w0 = sbt(es, "w0", [128, 1], F32)
                Hn = sbt(es, "Hn", [128, NIT + 1], F32)
                NHn = sbt(es, "NHn", [128, NIT + 1], F32)
                P2 = sbt(es, "P2", [128, NIT + 1], F32)
                NP2 = sbt(es, "NP2", [128, NIT + 1], F32)
                mid = sbt(es, "mid", [128, 1], F32)
                cnt = sbt(es, "cnt", [128, 1], F32)
                stp = sbt(es, "stp", [128, 1], F32)
                gm = sbt(es, "gm", [128, 128], F32)
                pmx = sbt(es, "pmx", [128, 8], F32)
                pmB = sbt(es, "pmB", [128, 128], F32)
                basef = sbt(es, "basef", [128, NH, 128], F32)
                bhi = sbt(es, "bhi", [128, NH, 128], BF16)
                rcp = sbt(es, "rcp", [128, 512], F32)
                oT = sbt(es, "oT", [128, 2, 512], BF16)
                attb = sbt(es, "attb", [128, NH, 128], BF16)
                dbt = sbt(es, "dbt", [128, 4], F32)
                px = [pst(es, f"px{i}", [128, 512]) for i in range(2)]
                psc = pst(es, "psc", [128, 512])
                pl = [pst(es, f"pl{i}", [128, 512]) for i in range(2)]
                po = [pst(es, f"po{i}", [128, 512]) for i in range(2)]
                prs = pst(es, "prs", [128, 512])

                SC.dma("pool", wuv[:], w_uv, writes=[wuv])
                for z_ in qz:
                    op("pool", lambda z_=z_: P.memset(z_[:], 0.0), writes=[z_])
                for n_ in range(NIT):
                    op("pool", lambda n_=n_: P.memset(P2[:, n_:n_ + 1], 2.0 ** -(n_ + 1)), writes=[P2])
                    e_ = n_ + 2 if n_ < NIT - 1 else n_ + 1
                    op("pool", lambda n_=n_, e_=e_: P.memset(NP2[:, n_:n_ + 1], -(2.0 ** -e_)), writes=[NP2])
                op("pool", lambda: P.memset(LPOS[:], 0.0), writes=[LPOS])
                op("pool", lambda: P.memset(LPOS[0:1, :, :], 1.0), reads=[LPOS], writes=[LPOS])
                op("pool", lambda: P.memset(LPOS[32:33, :, :], 1.0), reads=[LPOS], writes=[LPOS])
                op("pool", lambda: P.iota(LPOS[64:65, :, :], pattern=[[0, NB], [1, 128]], base=0, channel_multiplier=0,
                                          allow_small_or_imprecise_dtypes=True), reads=[LPOS], writes=[LPOS])
                op("pool", lambda: P.iota(LPOS[96:97, :, :], pattern=[[1, NB], [0, 128]], base=0, channel_multiplier=0,
                                          allow_small_or_imprecise_dtypes=True), reads=[LPOS], writes=[LPOS])
                for rb in RB:
                    op("pool", lambda rb=rb: P.memset(rb[:], 0.0), writes=[rb])
                    for hh in range(2):
                        for hl in range(4):
                            sl = 2.0 ** -(hh * 4 + hl + 1)
                            op("pool", lambda rb=rb, hh=hh, hl=hl, sl=sl: P.memset(
                                rb[64:65, hh, hl * 128:(hl + 1) * 128], sl), reads=[rb], writes=[rb])
                            op("pool", lambda rb=rb, hh=hh, hl=hl, sl=sl: P.memset(
                                rb[96:97, hh, hl * 128:(hl + 1) * 128], 128.0 * sl), reads=[rb], writes=[rb])

                def index_phase(i, part):
                    b = i % 2
                    L = (i + 1) * 128
                    q0 = i * 128
                    junkb = MB[b]
                    if part == "A":
                        index_A(i, b, L, q0)
                        bisect(L, junkb, 0, NIT // 2)
                    elif part == "B":
                        bisect(L, junkb, NIT // 2, NIT)
                        index_B(i, b, L)
                    else:
                        index_C(i, b, L)

                def bisect(L, junkb, n0, n1):
                    for n_ in range(n0, n1):
                        op("dve", lambda: V.tensor_scalar(out=junkb[:, 0:L], in0=score[:, 0:L], scalar1=mid[:],
                                                          scalar2=0.0, op0=ALU.is_ge, op1=ALU.add, accum_out=cnt[:]),
                           reads=[score, mid], writes=[junkb, cnt])
                        op("dve", lambda n_=n_: V.tensor_scalar(out=stp[:], in0=cnt[:], scalar1=TOPK - 0.5,
                                                                scalar2=Hn[:, n_:n_ + 1], op0=ALU.is_ge, op1=ALU.mult),
                           reads=[cnt, Hn], writes=[stp])
                        op("dve", lambda n_=n_: V.scalar_tensor_tensor(out=mid[:], in0=stp[:], scalar=NHn[:, n_:n_ + 1],
                                                                       in1=mid[:], op0=ALU.add, op1=ALU.add),
                           reads=[stp, NHn, mid], writes=[mid])

                def index_A(i, b, L, q0):
                    SC.dma("sp", qiblk[b][:], qi_s[:, :, q0:q0 + 128], writes=[qiblk[b]])
                    op("dve", lambda: V.scalar_tensor_tensor(
                        out=Dh[b][:], in0=ident_f[:].unsqueeze(1).to_broadcast([128, IH, 128]), scalar=IDX_SCALE,
                        in1=WI[:, i, :].unsqueeze(2).to_broadcast([128, IH, 128]), op0=ALU.mult, op1=ALU.mult),
                       reads=[ident_f, WI], writes=[Dh[b]])
                    qzv = qz[b][:].rearrange("p (a t) q -> p a t q", t=2)
                    op("pool", lambda: P.tensor_copy(out=qzv[0:64, :, 0, :], in_=qiblk[b][0:64, :, :]),
                       reads=[qiblk[b]], writes=[qz[b]])
                    op("pool", lambda: P.tensor_copy(out=qzv[64:128, :, 1, :], in_=qiblk[b][64:128, :, :]),
                       reads=[qiblk[b]], writes=[qz[b]])
                    nch = (L + 511) // 512
                    steps = [(c, h) for c in range(nch) for h in range(IH)]
                    pxs = [px[0], px[1], pl[0], pl[1]]
                    pscs = [psc, prs]

                    def emit_qk(k):
                        c, h = steps[k]
                        n = min(512, L - c * 512)
                        pp = pxs[k % 4]
                        p0 = (h % 2) * 64
                        MM(pp[:, 0:n], [(qz[b][:, h, :], kiT2[:, c * 512:c * 512 + n])], [qz[b], kiT2], [pp])

                    def emit_relu_acc(k):
                        c, h = steps[k]
                        n = min(512, L - c * 512)
                        pp = pxs[k % 4]
                        rh = Rh[k % 4]
                        pa = pscs[c % 2]
                        if k % 2 == 0:
                            op("act", lambda: A.activation(out=rh[:, 0:n], in_=pp[:, 0:n], func=AF.Relu),
                               reads=[pp], writes=[rh])
                        else:
                            op("dve", lambda: V.tensor_scalar(out=rh[:, 0:n], in0=pp[:, 0:n], scalar1=0.0, scalar2=None,
                                                              op0=ALU.max), reads=[pp], writes=[rh])
                        op("pe", lambda: T.matmul(pa[:, 0:n], lhsT=Dh[b][:, h, :], rhs=rh[:, 0:n], start=(h == 0),
                                                  stop=(h == IH - 1)),
                           reads=[Dh[b], rh] + ([pa] if h == 0 else []), writes=[pa])
                        if h == IH - 1:
                            op("act", lambda: A.copy(out=score[:, c * 512:c * 512 + n], in_=pa[:, 0:n]),
                               reads=[pa], writes=[score])

                    LOOK = 2
                    for k in range(min(LOOK, len(steps))):
                        emit_qk(k)
                    for k in range(len(steps)):
                        if k + LOOK < len(steps):
                            emit_qk(k + LOOK)
                        emit_relu_acc(k)
                    op("dve", lambda: V.tensor_tensor(out=tmpd[:], in0=score[:, L - 128:L], in1=cpos[:], op=ALU.add),
                       reads=[score, cpos], writes=[tmpd])
                    op("dve", lambda: V.tensor_reduce(out=rmin[:, 0:1], in_=tmpd[:], axis=AX.X, op=ALU.min),
                       reads=[tmpd], writes=[rmin])
                    if L > 128:
                        op("dve", lambda: V.tensor_reduce(out=rmin[:, 1:2], in_=score[:, 0:L - 128], axis=AX.X,
                                                          op=ALU.min), reads=[score, rmin], writes=[rmin])
                        op("dve", lambda: V.tensor_tensor(out=lo[:], in0=rmin[:, 0:1], in1=rmin[:, 1:2], op=ALU.min),
                           reads=[rmin], writes=[lo])
                    else:
                        op("dve", lambda: V.tensor_copy(out=lo[:], in_=rmin[:, 0:1]), reads=[rmin], writes=[lo])
                    op("dve", lambda: V.tensor_tensor(out=score[:, L - 128:L], in0=score[:, L - 128:L], in1=caus[:],
                                                      op=ALU.add), reads=[score, caus], writes=[score])
                    op("dve", lambda: V.max(out=m8[:], in_=score[:, 0:L]), reads=[score], writes=[m8])
                    op("dve", lambda: V.tensor_tensor(out=w0[:], in0=m8[:, 0:1], in1=lo[:], op=ALU.subtract),
                       reads=[m8, lo], writes=[w0])
                    op("dve", lambda: V.tensor_scalar(out=Hn[:, 0:NIT], in0=P2[:, 0:NIT], scalar1=w0[:], scalar2=None,
                                                      op0=ALU.mult), reads=[w0, P2], writes=[Hn])
                    op("dve", lambda: V.tensor_scalar(out=NHn[:, 0:NIT], in0=NP2[:, 0:NIT], scalar1=w0[:], scalar2=None,
                                                      op0=ALU.mult), reads=[w0, NP2], writes=[NHn])
                    op("dve", lambda: V.tensor_tensor(out=mid[:], in0=lo[:], in1=Hn[:, 0:1], op=ALU.add),
                       reads=[lo, Hn], writes=[mid])

                def index_B(i, b, L):
                    op("dve", lambda: V.tensor_scalar(out=MB[b][:, 0:L], in0=score[:, 0:L], scalar1=mid[:], scalar2=NEG,
                                                      op0=ALU.is_lt, op1=ALU.mult), reads=[score, mid], writes=[MB[b]])
                    if debug:
                        SC.dma("sp", dbg["score"][i, :, 0:L], score[:, 0:L], reads=[score])
                        op("dve", lambda: V.tensor_copy(out=dbt[:, 0:1], in_=mid[:]), reads=[mid], writes=[dbt])
                        op("dve", lambda: V.tensor_copy(out=dbt[:, 1:2], in_=cnt[:]), reads=[cnt], writes=[dbt])
                    ng = L // 64
                    op("dve", lambda: V.tensor_reduce(out=gm[:, 0:ng],
                                                      in_=MB[b][:, 0:L].rearrange("p (g k) -> p g k", k=64),
                                                      axis=AX.X, op=ALU.max), reads=[MB[b]], writes=[gm])
                    op("dve", lambda: V.scalar_tensor_tensor(out=gm[:, 0:ng], in0=gm[:, 0:ng], scalar=-1.0,
                                                             in1=gpos[:, 0:ng], op0=ALU.is_ge, op1=ALU.mult),
                       reads=[gm, gpos], writes=[gm])
                    op("dve", lambda: V.tensor_reduce(out=pmx[:, 0:1], in_=gm[:, 0:ng], axis=AX.X, op=ALU.max),
                       reads=[gm], writes=[pmx])
                    if debug:
                        op("dve", lambda: V.tensor_copy(out=dbt[:, 2:3], in_=pmx[:, 0:1]), reads=[pmx], writes=[dbt])
                        SC.dma("sp", dbg["thr"][:, i, :], dbt[:], reads=[dbt])
                    op("dve", lambda: V.tensor_copy(out=pmB[:], in_=pmx[:, 0:1].to_broadcast([128, 128])),
                       reads=[pmx], writes=[pmB])

                def index_C(i, b, L):
                    MM(psc[:, 0:128], [(pmB[:], ident_f[:])], [pmB, ident_f], [psc])
                    op("dve", lambda: V.tensor_tensor(
                        out=basef[:], in0=psc[:, 0:128].unsqueeze(1).to_broadcast([128, NH, 128]),
                        in1=nsl[:].unsqueeze(2).to_broadcast([128, NH, 128]), op=ALU.mult),
                       reads=[psc, nsl], writes=[basef])
                    op("dve", lambda: V.tensor_copy(out=bhi[:], in_=basef[:]), reads=[basef], writes=[bhi])
                    op("dve", lambda: V.tensor_tensor(out=basef[:], in0=basef[:], in1=bhi[:], op=ALU.subtract),
                       reads=[basef, bhi], writes=[basef])
                    op("dve", lambda: V.tensor_copy(out=RB[b][0:1, :, :].rearrange("p a (h q) -> p (a h) q", h=4),
                                                    in_=bhi[0:1, :, :]), reads=[bhi], writes=[RB[b]])
                    op("dve", lambda: V.tensor_copy(out=RB[b][32:33, :, :].rearrange("p a (h q) -> p (a h) q", h=4),
                                                    in_=basef[32:33, :, :]), reads=[basef], writes=[RB[b]])

                def attn_phase(i, hsel):
                    b = i % 2
                    q0 = i * 128
                    if hsel == 0:
                        SC.dma("sp", qlblk[b][:], qlat_s[:, :, :, q0:q0 + 128], writes=[qlblk[b]])
                    items = [(hsel, j) for j in range(i + 1)]

                    def emit_L(k):
                        hh, j = items[k]
                        pp = pl[k % 2]
                        terms = [(CT[:, rc, j * 128:(j + 1) * 128],
                                  qlblk[b][:, rc, hh * 4:hh * 4 + 4, :].rearrange("p h q -> p (h q)"))
                                 for rc in range(2)]
                        terms.append((MB[b][:, j * 128:(j + 1) * 128], irep[:].rearrange("p r q -> p (r q)")))
                        terms.append((LPOS[:, j, :], RB[b][:, hh, :]))
                        MM(pp[:], terms, [CT, qlblk[b], MB[b], irep, LPOS, RB[b]], [pp])

                    def emit_rest(k):
                        hh, j = items[k]
                        pp = pl[k % 2]
                        pt = PT[k % 3]
                        op("act", lambda: A.activation(out=pt[:], in_=pp[:], func=AF.Exp), reads=[pp], writes=[pt])

                        def pv():
                            ins = None
                            for rc in range(2):
                                ins = T.matmul(po[rc][:], lhsT=Cc[:, j, rc * 128:(rc + 1) * 128], rhs=pt[:],
                                               start=(j == 0), stop=(j == i))
                            ins = T.matmul(prs[:], lhsT=ones_b[:], rhs=pt[:], start=(j == 0), stop=(j == i))
                            return ins
                        op("pe", pv, reads=[Cc, pt, ones_b] + ([po[0], po[1], prs] if j == 0 else []),
                           writes=[po[0], po[1], prs])
                        if j != i:
                            return
                        op("dve", lambda: V.reciprocal(out=rcp[:], in_=prs[:]), reads=[prs], writes=[rcp])
                        for rc in range(2):
                            op("dve", lambda rc=rc: V.tensor_tensor(out=oT[:, rc, :], in0=po[rc][:], in1=rcp[:],
                                                                    op=ALU.mult), reads=[po[rc], rcp], writes=[oT])
                        pa = px[hh]

                        def av():
                            ins = None
                            for hl in range(4):
                                h = hh * 4 + hl
                                for rc in range(2):
                                    ins = T.matmul(pa[:, hl * 128:(hl + 1) * 128], lhsT=wuv[:, rc, h * 128:(h + 1) * 128],
                                                   rhs=oT[:, rc, hl * 128:(hl + 1) * 128], start=(rc == 0), stop=(rc == 1))
                            return ins
                        op("pe", av, reads=[wuv, oT], writes=[pa])
                        op("act", lambda: A.copy(out=attb[:, hh * 4:hh * 4 + 4, :].rearrange("p h q -> p (h q)"),
                                                 in_=pa[:]), reads=[pa], writes=[attb])

                    emit_L(0)
                    for k in range(len(items)):
                        if k + 1 < len(items):
                            emit_L(k + 1)
                        emit_rest(k)
                    if hsel == 1:
                        SC.dma("sp", att_s[:, :, q0:q0 + 128], attb[:], reads=[attb])

                for i in range(NB + 1):
                    if i < NB:
                        index_phase(i, "A")
                    if i >= 1:
                        attn_phase(i - 1, 0)
                    if i < NB:
                        index_phase(i, "B")
                    if i >= 1:
                        attn_phase(i - 1, 1)
                    if i < NB:
                        index_phase(i, "C")
                SC.barrier()

        WT = sbt(es0, "WT", [128, NB, NE], F32)
        with ExitStack() as es:
            g1row = sbt(es, "g1row", [128, D], F32)
            wat = sbt(es, "wat", [128, 8, D], BF16)
            wo = sbt(es, "wo", [128, 8, D], BF16)
            wge = sbt(es, "wge", [128, 8, 36], F32)
            bge = sbt(es, "bge", [128, 36], F32)
            attT = [sbt(es, f"attT{i}", [128, 8, 512], BF16) for i in range(2)]
            sgT = [sbt(es, f"sgT{i}", [128, 16, 512], BF16) for i in range(2)]
            yaT = [sbt(es, f"yaT{i}", [128, 8, 512], BF16) for i in range(2)]
            t1 = [sbt(es, f"t1_{i}", [128, 512], F32) for i in range(2)]
            t2 = [sbt(es, f"t2_{i}", [128, 512], F32) for i in range(2)]
            mixin = sbt(es, "mixin", [128, 8, 512], BF16)
            xs = [sbt(es, f"xsB{i}", [128, D], F32) for i in range(2)]
            hb = [sbt(es, f"hb{i}", [128, D], F32) for i in range(2)]
            u2 = [sbt(es, f"u2_{i}", [128, D], F32) for i in range(2)]
            junk = sbt(es, "junkB2", [128, D], F32)
            ssq = [sbt(es, f"ssqB{i}", [128, 1], F32) for i in range(2)]
            rs = [sbt(es, f"rsB{i}", [128, 1], F32) for i in range(2)]
            u2Tf = sbt(es, "u2Tf", [128, 8, 128], F32)
            u2Tb = sbt(es, "u2Tb", [128, 8, 512], BF16)
            lg = sbt(es, "lg", [128, 36], F32)
            em = sbt(es, "em", [128, NE], F32)
            sm = sbt(es, "sm", [128, 16], F32)
            m8 = sbt(es, "m8r", [128, 8], F32)
            wta = sbt(es, "wta", [128, NE], F32)
            wtb = sbt(es, "wtb", [128, NE], F32)
            pyb = [pst(es, f"pyb{i}", [128, 512]) for i in range(2)]
            pmx = pst(es, "pmxo", [128, 2, 512])
            ptf = pst(es, "ptf", [128, 8, 128])
            plg = pst(es, "plg", [128, 512])
            mod_rows(g1row, 4, plg)
            wload(wat, w_att)
            wload(wo, w_o)
            SC.dma("sp", wge[:], w_ge, writes=[wge])
            SC.dma("sp", bge[:], b_ge_rep, writes=[bge])
            for t in range(NT):
                t0 = t * 512
                b = t % 2
                SC.dma("sp", attT[b][:], att_s[:, :, t0:t0 + 512], writes=[attT[b]])
                SC.dma("sp", sgT[b][:], sg_s[:, :, t0:t0 + 512], writes=[sgT[b]])
                SC.dma("sp", yaT[b][:], ya_s[:, :, t0:t0 + 512], writes=[yaT[b]])
                for dc in range(8):
                    pp = pyb[dc % 2]
                    d2 = dc % 2
                    MM(pp[:], [(wat[:, h, dc * 128:(dc + 1) * 128], attT[b][:, h, :]) for h in range(8)],
                       [wat, attT[b]], [pp])
                    op("dve", lambda pp=pp, dc=dc, d2=d2: V.tensor_tensor(out=t2[d2][:], in0=sgT[b][:, 8 + dc, :], in1=pp[:],
                                                                          op=ALU.mult), reads=[sgT[b], pp], writes=[t2[d2]])
                    op("pool", lambda dc=dc, d2=d2: P.tensor_tensor(out=t1[d2][:], in0=sgT[b][:, dc, :], in1=yaT[b][:, dc, :],
                                                                    op=ALU.mult), reads=[sgT[b], yaT[b]], writes=[t1[d2]])
                    op("pool", lambda dc=dc, d2=d2: P.tensor_tensor(out=mixin[:, dc, :], in0=t1[d2][:], in1=t2[d2][:],
                                                                    op=ALU.add), reads=[t1[d2], t2[d2]], writes=[mixin])
                def stageA(s):
                    blk = t * 4 + s
                    sb2 = s % 2
                    r0 = t0 + s * 128
                    hcur = hb[sb2]
                    SC.dma("sp", xs[sb2][:], x[r0:r0 + 128, :], writes=[xs[sb2]])
                    def mo(s=s):
                        ins = None
                        for half in range(2):
                            for kc in range(8):
                                ins = T.matmul(pmx[:, half, :], lhsT=mixin[:, kc, s * 128:(s + 1) * 128],
                                               rhs=wo[:, kc, half * 512:(half + 1) * 512], start=(kc == 0), stop=(kc == 7))
                        return ins
                    op("pe", mo, reads=[mixin, wo], writes=[pmx])
                    hcur = hb[sb2]
                    op("dve", lambda hcur=hcur: V.tensor_tensor(out=hcur[:], in0=pmx[:].rearrange("p a b -> p (a b)"),
                                                                in1=g1row[:], op=ALU.mult), reads=[pmx, g1row], writes=[hcur])
                    op("pool", lambda hcur=hcur, sb2=sb2: P.tensor_tensor(out=hcur[:], in0=hcur[:], in1=xs[sb2][:],
                                                                          op=ALU.add), reads=[hcur, xs[sb2]], writes=[hcur])
                    SC.dma("sp", h_s[r0:r0 + 128, :], hcur[:], reads=[hcur])
                    op("act", lambda hcur=hcur, sb2=sb2: A.activation(out=junk[:], in_=hcur[:], func=AF.Square,
                                                                      accum_out=ssq[sb2][:]),
                       reads=[hcur], writes=[junk, ssq[sb2]])
                    op("act", lambda sb2=sb2: A.activation(out=rs[sb2][:], in_=ssq[sb2][:], func=AF.Sqrt, bias=eps_c[:],
                                                           scale=1.0 / D), reads=[ssq[sb2], eps_c], writes=[rs[sb2]])
                    op("dve", lambda sb2=sb2: V.reciprocal(out=rs[sb2][:], in_=rs[sb2][:]), reads=[rs[sb2]], writes=[rs[sb2]])
                    op("dve", lambda hcur=hcur, sb2=sb2: V.tensor_scalar(out=u2[sb2][:], in0=hcur[:], scalar1=rs[sb2][:],
                                                                         scalar2=None, op0=ALU.mult),
                       reads=[hcur, rs[sb2]], writes=[u2[sb2]])

                def stageB(s):
                    blk = t * 4 + s
                    sb2 = s % 2
                    r0 = t0 + s * 128
                    hcur = hb[sb2]
                    def tr(sb2=sb2):
                        ins = None
                        for c in range(8):
                            ins = T.transpose(out=ptf[:, c, :], in_=u2[sb2][:, c * 128:(c + 1) * 128], identity=ident_f[:])
                        return ins
                    op("pe", tr, reads=[u2[sb2], ident_f], writes=[ptf])
                    for c in range(8):
                        op("act", lambda c=c: A.activation(out=u2Tf[:, c, :], in_=ptf[:, c, :], func=AF.Identity,
                                                           bias=B2[:, c:c + 1], scale=A2[:, c:c + 1]),
                           reads=[ptf, A2, B2], writes=[u2Tf])
                    op("dve", lambda s=s: V.tensor_copy(out=u2Tb[:, :, s * 128:(s + 1) * 128], in_=u2Tf[:]),
                       reads=[u2Tf], writes=[u2Tb])
                    MM(plg[:, 0:36], [(u2Tf[:, kc, :], wge[:, kc, :]) for kc in range(8)], [u2Tf, wge], [plg])
                    op("dve", lambda: V.tensor_tensor(out=lg[:], in0=plg[:, 0:36], in1=bge[:], op=ALU.add),
                       reads=[plg, bge], writes=[lg])
                    op("dve", lambda: V.tensor_reduce(out=sm[:, 0:1], in_=lg[:, 0:NG], axis=AX.X, op=ALU.max),
                       reads=[lg], writes=[sm])
                    op("dve", lambda: V.tensor_scalar(out=sm[:, 1:2], in0=sm[:, 0:1], scalar1=-1.0, scalar2=None,
                                                      op0=ALU.mult), reads=[sm], writes=[sm])
                    op("act", lambda: A.activation(out=sm[:, 4:8], in_=lg[:, 0:NG], func=AF.Exp, bias=sm[:, 1:2],
                                                   scale=1.0, accum_out=sm[:, 2:3]), reads=[lg, sm], writes=[sm])
                    op("dve", lambda: V.reciprocal(out=sm[:, 3:4], in_=sm[:, 2:3]), reads=[sm], writes=[sm])
                    op("dve", lambda: V.tensor_scalar(out=sm[:, 8:12], in0=lg[:, 0:NG], scalar1=sm[:, 0:1], scalar2=-1e9,
                                                      op0=ALU.is_lt, op1=ALU.mult), reads=[lg, sm], writes=[sm])
                    for g in range(NG):
                        op("dve", lambda g=g: V.tensor_scalar(out=em[:, g * EPG:(g + 1) * EPG],
                                                              in0=lg[:, NG + g * EPG:NG + (g + 1) * EPG],
                                                              scalar1=sm[:, 8 + g:9 + g], scalar2=None, op0=ALU.add),
                           reads=[lg, sm], writes=[em])
                    op("dve", lambda: V.max(out=m8[:], in_=em[:]), reads=[em], writes=[m8])
                    op("dve", lambda: V.tensor_tensor(out=sm[:, 12:13], in0=m8[:, 1:2], in1=m8[:, 0:1], op=ALU.subtract),
                       reads=[m8], writes=[sm])
                    op("act", lambda: A.activation(out=sm[:, 13:14], in_=sm[:, 12:13], func=AF.Exp), reads=[sm], writes=[sm])
                    op("dve", lambda: V.tensor_scalar(out=sm[:, 14:15], in0=sm[:, 13:14], scalar1=1.0, scalar2=None,
                                                      op0=ALU.add), reads=[sm], writes=[sm])
                    op("dve", lambda: V.reciprocal(out=sm[:, 14:15], in_=sm[:, 14:15]), reads=[sm], writes=[sm])
                    op("dve", lambda: V.tensor_tensor(out=sm[:, 14:15], in0=sm[:, 14:15], in1=sm[:, 3:4], op=ALU.mult),
                       reads=[sm], writes=[sm])
                    op("dve", lambda: V.tensor_tensor(out=sm[:, 15:16], in0=sm[:, 14:15], in1=sm[:, 13:14], op=ALU.mult),
                       reads=[sm], writes=[sm])
                    op("dve", lambda: V.tensor_scalar(out=wta[:], in0=em[:], scalar1=m8[:, 0:1], scalar2=sm[:, 14:15],
                                                      op0=ALU.is_equal, op1=ALU.mult), reads=[em, m8, sm], writes=[wta])
                    op("dve", lambda: V.tensor_scalar(out=wtb[:], in0=em[:], scalar1=m8[:, 1:2], scalar2=sm[:, 15:16],
                                                      op0=ALU.is_equal, op1=ALU.mult), reads=[em, m8, sm], writes=[wtb])
                    op("dve", lambda blk=blk: V.tensor_tensor(out=WT[:, blk, :], in0=wta[:], in1=wtb[:], op=ALU.add),
                       reads=[wta, wtb], writes=[WT])

                stageA(0)
                for s in range(4):
                    if s + 1 < 4:
                        stageA(s + 1)
                    stageB(s)
                SC.dma("sp", u2_s[:, :, t0:t0 + 512], u2Tb[:], reads=[u2Tb])
            if debug:
                SC.dma("sp", dbg["wt"], WT[:], reads=[WT])
            SC.barrier()

        with ExitStack() as es:
            NSB = TC // 128
            NHF = TC // 512
            g2row = sbt(es, "g2row", [128, D], F32)
            ph1 = [pst(es, f"ph1_{i}", [128, 512]) for i in range(2)]
            mod_rows(g2row, 10, ph1[0])
            w1b = [sbt(es, f"w1b{i}", [128, 8, DE], BF16) for i in range(2)]
            w3b = [sbt(es, f"w3b{i}", [128, 8, DE], BF16) for i in range(2)]
            w2b = [sbt(es, f"w2b{i}", [128, 4, D], BF16) for i in range(2)]
            u2T = sbt(es, "u2T", [128, 8, TC], BF16)
            acc = sbt(es, "acc", [128, NSB, D], F32)
            s1 = [sbt(es, f"s1_{i}", [128, 512], F32) for i in range(2)]
            hid = [sbt(es, f"hid{i}", [128, 4, 512], BF16) for i in range(2)]
            hh_ = [sbt(es, f"hC{i}", [128, D], F32) for i in range(2)]
            junk = sbt(es, "junkC", [128, D], F32)
            ssq = [sbt(es, f"ssqC{i}", [128, 1], F32) for i in range(2)]
            rs = [sbt(es, f"rsC{i}", [128, 1], F32) for i in range(2)]
            fg = sbt(es, "fg", [128, D], F32)
            ph3 = [pst(es, f"ph3_{i}", [128, 512]) for i in range(2)]
            pye = [pst(es, f"pye{i}", [128, 2, 512]) for i in range(2)]
            SC.dma("sp", fg[:], fg_rep, writes=[fg])
            kk = 0
            for tcn in range(NTC):
                c0 = tcn * TC
                SC.dma("sp", u2T[:], u2_s[:, :, c0:c0 + TC], writes=[u2T])
                for e in range(NE):
                    wb = e % 2
                    SC.dma("pool", w1b[wb][:], moe_w1[e].rearrange("(kc p) f -> p kc f", p=128), writes=[w1b[wb]])
                    SC.dma("pool", w3b[wb][:], moe_w3[e].rearrange("(kc p) f -> p kc f", p=128), writes=[w3b[wb]])
                    SC.dma("pool", w2b[wb][:], moe_w2[e].rearrange("(fc p) d -> p fc d", p=128), writes=[w2b[wb]])
                    for hf in range(NHF):
                        hd_ = hid[hf % 2]
                        for fc in range(4):
                            p1 = ph1[fc % 2]
                            p3 = ph3[fc % 2]
                            MM(p1[:], [(w1b[wb][:, kc, fc * 128:(fc + 1) * 128], u2T[:, kc, hf * 512:(hf + 1) * 512])
                                       for kc in range(8)], [w1b[wb], u2T], [p1])
                            MM(p3[:], [(w3b[wb][:, kc, fc * 128:(fc + 1) * 128], u2T[:, kc, hf * 512:(hf + 1) * 512])
                                       for kc in range(8)], [w3b[wb], u2T], [p3])
                            sb_ = s1[fc % 2]
                            op("act", lambda p1=p1, sb_=sb_: A.activation(out=sb_[:], in_=p1[:], func=AF.Silu),
                               reads=[p1], writes=[sb_])
                            op("dve", lambda p3=p3, sb_=sb_, hd_=hd_, fc=fc: V.tensor_tensor(
                                out=hd_[:, fc, :], in0=sb_[:], in1=p3[:], op=ALU.mult), reads=[sb_, p3], writes=[hd_])
                        for sub in range(4):
                            pe_ = pye[kk % 2]
                            kk += 1
                            si = hf * 4 + sub
                            blk = (c0 // 128) + si
                            def my(sub=sub, pe_=pe_, hd_=hd_, wb=wb):
                                ins = None
                                for dh in range(2):
                                    for fc in range(4):
                                        ins = T.matmul(pe_[:, dh, :], lhsT=hd_[:, fc, sub * 128:(sub + 1) * 128],
                                                       rhs=w2b[wb][:, fc, dh * 512:(dh + 1) * 512],
                                                       start=(fc == 0), stop=(fc == 3))
                                return ins
                            op("pe", my, reads=[hd_, w2b[wb]], writes=[pe_])
                            if e == 0:
                                op("dve", lambda pe_=pe_, si=si, blk=blk, e=e: V.tensor_scalar(
                                    out=acc[:, si, :], in0=pe_[:].rearrange("p a b -> p (a b)"),
                                    scalar1=WT[:, blk, e:e + 1], scalar2=None, op0=ALU.mult),
                                   reads=[pe_, WT], writes=[acc])
                            else:
                                op("dve", lambda pe_=pe_, si=si, blk=blk, e=e: V.scalar_tensor_tensor(
                                    out=acc[:, si, :], in0=pe_[:].rearrange("p a b -> p (a b)"),
                                    scalar=WT[:, blk, e:e + 1], in1=acc[:, si, :], op0=ALU.mult, op1=ALU.add),
                                   reads=[pe_, WT, acc], writes=[acc])
                for si in range(NSB):
                    b = si % 2
                    r0 = c0 + si * 128
                    hc = hh_[b]
                    SC.dma("sp", hc[:], h_s[r0:r0 + 128, :], writes=[hc])
                    op("pool", lambda si=si: P.tensor_tensor(out=acc[:, si, :], in0=acc[:, si, :], in1=g2row[:],
                                                             op=ALU.mult), reads=[acc, g2row], writes=[acc])
                    op("pool", lambda si=si, hc=hc: P.tensor_tensor(out=hc[:], in0=hc[:], in1=acc[:, si, :], op=ALU.add),
                       reads=[hc, acc], writes=[hc])
                    op("act", lambda hc=hc, b=b: A.activation(out=junk[:], in_=hc[:], func=AF.Square,
                                                              accum_out=ssq[b][:]), reads=[hc], writes=[junk, ssq[b]])
                    op("act", lambda b=b: A.activation(out=rs[b][:], in_=ssq[b][:], func=AF.Sqrt, bias=eps_c[:],
                                                       scale=1.0 / D), reads=[ssq[b], eps_c], writes=[rs[b]])
                    op("dve", lambda b=b: V.reciprocal(out=rs[b][:], in_=rs[b][:]), reads=[rs[b]], writes=[rs[b]])
                    op("dve", lambda hc=hc, b=b: V.scalar_tensor_tensor(out=hc[:], in0=hc[:], scalar=rs[b][:], in1=fg[:],
                                                                        op0=ALU.mult, op1=ALU.mult),
                       reads=[hc, rs[b], fg], writes=[hc])
                    SC.dma("sp", out[r0:r0 + 128, :], hc[:], reads=[hc])
            SC.barrier()
    SC.close()
    return nc


def _col(v, n):
    return np.ascontiguousarray(np.asarray(v, np.float32).reshape(n, 128).T)


def _kc(w):
    K, N = w.shape
    return np.ascontiguousarray(np.asarray(w, np.float32).reshape(K // 128, 128, N).transpose(1, 0, 2))


def _rep(v):
    v = np.asarray(v, np.float32).reshape(1, -1)
    return np.ascontiguousarray(np.broadcast_to(v, (128, v.shape[1])))


def prep_inputs(inp, b):
    l = 0
    w_in = np.asarray(inp["w_in"][l], np.float32)
    o = np.cumsum([0, DR, DR, NH * HD, R, IH * IDIM, IDIM, IH, 2 * D])
    xr, gr, q, ckv, qi, ki, wi, mg = [w_in[:, o[i]:o[i + 1]] for i in range(8)]
    m = {}
    m["x"] = np.ascontiguousarray(inp["x"][b], dtype=np.float32)
    m["c_col"] = _col(inp["c"][b], 8)
    m["w_mod"] = _kc(inp["w_mod"][l])
    m["b_mod_col"] = _col(inp["b_mod"][l], 48)
    m["b_mod_rep"] = _rep(inp["b_mod"][l])
    m["n1_col"] = _col(inp["norm1_g"][l], 8)
    m["n2_col"] = _col(inp["norm2_g"][l], 8)
    m["w_fm1"] = _kc(np.concatenate([xr, gr, mg], axis=1))
    m["w_fm2"] = _kc(np.concatenate([q, qi, ki, ki], axis=1))
    m["w_tm"] = _kc(np.concatenate([ckv, wi], axis=1))
    m["conv_w"] = np.ascontiguousarray(np.asarray(inp["conv_w"][l], np.float32).reshape(4, NRB, 128).transpose(2, 1, 0))
    m["conv_b"] = _col(inp["conv_b"][l], NRB)
    m["w_rg_a"] = np.ascontiguousarray(np.asarray(inp["w_rg_a"][l], np.float32).transpose(1, 0, 2))
    m["w_rg_x"] = np.ascontiguousarray(np.asarray(inp["w_rg_x"][l], np.float32).transpose(1, 0, 2))
    m["b_rg_a"] = _col(inp["b_rg_a"][l], NRB)
    m["b_rg_x"] = _col(inp["b_rg_x"][l], NRB)
    m["lam"] = _col(inp["lru_lambda"][l], NRB)
    m["gkv_rep"] = _rep(inp["kv_norm_g"][l])
    m["w_ukT"] = np.ascontiguousarray(np.asarray(inp["w_uk"][l], np.float32).transpose(2, 1, 0))
    m["w_uv"] = _kc(np.asarray(inp["w_uv"][l], np.float32).reshape(R, NH * HD))
    m["w_rnn"] = _kc(inp["w_rnn_out"][l])
    m["w_att"] = _kc(inp["w_att_out"][l])
    m["w_o"] = _kc(inp["w_out"][l])
    m["w_ge"] = _kc(np.concatenate([np.asarray(inp["w_group"][l]), np.asarray(inp["w_expert"][l])], axis=1))
    m["b_ge_rep"] = _rep(np.concatenate([np.asarray(inp["b_group"][l]), np.asarray(inp["b_expert"][l])]))
    m["moe_w1"] = np.ascontiguousarray(inp["moe_w1"][l], dtype=np.float32)
    m["moe_w3"] = np.ascontiguousarray(inp["moe_w3"][l], dtype=np.float32)
    m["moe_w2"] = np.ascontiguousarray(inp["moe_w2"][l], dtype=np.float32)
    m["fg_rep"] = _rep(inp["final_g"])
    return m


def kernel(**inputs):
    x = np.asarray(inputs["x"])
    B, S, _ = x.shape
    nc = build(S)
    shared = None
    in_maps = []
    for b in range(B):
        m = prep_inputs(inputs, b)
        if shared is None:
            shared = m
        else:
            for k in m:
                if k not in ("x", "c_col"):
                    m[k] = shared[k]
        in_maps.append(m)
    res = run_bass_kernel_spmd(nc, in_maps, core_ids=list(range(B)))
    return np.stack([np.asarray(r["out"], dtype=np.float32) for r in res.results], axis=0)
```

```python
import numpy as np
from contextlib import ExitStack
import concourse.bass as bass
import concourse.mybir as mybir
from concourse.bass_utils import run_bass_kernel_spmd

F32 = mybir.dt.float32
BF16 = mybir.dt.bfloat16
AF = mybir.ActivationFunctionType
ALU = mybir.AluOpType
AX = mybir.AxisListType

D = 1024
DR = 1280
NRB = 10
NH = 8
HD = 128
R = 256
IH = 8
IDIM = 64
TOPK = 256
NE = 32
NG = 4
EPG = 8
DE = 512
EPS = 1e-6
IDX_SCALE = IDIM ** -0.5 * IH ** -0.5
ATT_SCALE = HD ** -0.5
NEG = -30000.0
NIT = 16
FM1 = 2560 + 2048
FM2 = 1024 + 512 + 128
TMC = 264


class Sched:
    def __init__(self, nc, n_dma_slots=10):
        self.nc = nc
        self.eng = {"pe": nc.tensor, "act": nc.scalar, "dve": nc.vector, "pool": nc.gpsimd, "sp": nc.sync}
        self.sem, self.cnt, self._cms = {}, {}, []
        for e in self.eng:
            cm = nc.semaphore("prog_" + e)
            self.sem[e] = cm.__enter__()
            self._cms.append(cm)
            self.cnt[e] = 0
        self.dma_q = ("sp", "pool")
        self.dslots = {}
        for q in self.dma_q:
            sl = []
            for i in range(n_dma_slots):
                cm = nc.semaphore(f"dma_{q}_{i}")
                sl.append([cm.__enter__(), 0])
                self._cms.append(cm)
            self.dslots[q] = sl
        self.dnext = {q: 0 for q in self.dma_q}
        self.waited = {e: {} for e in self.eng}
        self.state = {}

    def close(self):
        for cm in reversed(self._cms):
            cm.__exit__(None, None, None)

    @staticmethod
    def _key(t):
        return t if isinstance(t, str) else id(t)

    def _deps(self, reads, writes):
        deps = []
        for t in reads:
            st = self.state.get(self._key(t))
            if st and st[0] is not None:
                deps.append(st[0])
        for t in writes:
            st = self.state.get(self._key(t))
            if st:
                if st[0] is not None:
                    deps.append(st[0])
                deps.extend(st[1])
        return deps

    def _wait(self, e, deps):
        w = self.waited[e]
        best = {}
        for (sname, sem, val) in deps:
            if e == "pe" and sname == "prog_pe":
                continue
            if w.get(sname, 0) >= val:
                continue
            if sname not in best or best[sname][1] < val:
                best[sname] = (sem, val)
        for sname, (sem, val) in best.items():
            self.eng[e].wait_ge(sem, val)
            w[sname] = val

    def _record(self, tok, reads, writes):
        for t in writes:
            self.state[self._key(t)] = [tok, []]
        for t in reads:
            st = self.state.setdefault(self._key(t), [None, []])
            st[1] = [x for x in st[1] if x[0] != tok[0]] + [tok]

    def op(self, e, fn, reads=(), writes=()):
        self._wait(e, self._deps(reads, writes))
        ins = fn()
        self.cnt[e] += 1
        ins.then_inc(self.sem[e], 1)
        tok = ("prog_" + e, self.sem[e], self.cnt[e])
        self._record(tok, reads, writes)
        return tok

    def dma(self, q, out, in_, reads=(), writes=()):
        sl = self.dslots[q]
        i = self.dnext[q]
        self.dnext[q] = (i + 1) % len(sl)
        sem, used = sl[i]
        sname = f"dma_{q}_{i}"
        deps = self._deps(reads, writes)
        if used:
            deps.append((sname, sem, used))
        self._wait(q, deps)
        self.eng[q].dma_start(out=out, in_=in_).then_inc(sem, 16)
        sl[i][1] = used + 16
        tok = (sname, sem, used + 16)
        self._record(tok, reads, writes)
        return tok

    def barrier(self):
        toks = [("prog_" + e, self.sem[e], self.cnt[e]) for e in self.eng if self.cnt[e]]
        for q in self.dma_q:
            for i, (sem, used) in enumerate(self.dslots[q]):
                if used:
                    toks.append((f"dma_{q}_{i}", sem, used))
        for e in self.eng:
            self._wait(e, toks)
        self.state = {}


def build(S, debug=False):
    NB = S // 128
    NT = S // 512
    TC = min(1024, S)
    NTC = S // TC
    nc = bass.Bass("TRN2", target_bir_lowering=False)

    def din(name, shape, dt=F32):
        return nc.dram_tensor(name, list(shape), dt, kind="ExternalInput").ap()

    def dscr(name, shape, dt):
        return nc.dram_tensor(name, list(shape), dt, kind="ExternalOutput" if debug else "Internal").ap()

    x = din("x", [S, D])
    c_col = din("c_col", [128, 8])
    w_mod = din("w_mod", [128, 8, 6 * D])
    b_mod_col = din("b_mod_col", [128, 48])
    b_mod_rep = din("b_mod_rep", [128, 6 * D])
    n1_col = din("n1_col", [128, 8])
    n2_col = din("n2_col", [128, 8])
    w_fm1 = din("w_fm1", [128, 8, FM1])
    w_fm2 = din("w_fm2", [128, 8, FM2])
    w_tm = din("w_tm", [128, 8, TMC])
    conv_w = din("conv_w", [128, NRB, 4])
    conv_b = din("conv_b", [128, NRB])
    w_rg_a = din("w_rg_a", [128, NRB, 128])
    w_rg_x = din("w_rg_x", [128, NRB, 128])
    b_rg_a = din("b_rg_a", [128, NRB])
    b_rg_x = din("b_rg_x", [128, NRB])
    lam = din("lam", [128, NRB])
    gkv_rep = din("gkv_rep", [128, R])
    w_ukT = din("w_ukT", [128, NH, R])
    w_uv = din("w_uv", [128, 2, NH * HD])
    w_rnn = din("w_rnn", [128, NRB, D])
    w_att = din("w_att", [128, 8, D])
    w_o = din("w_o", [128, 8, D])
    w_ge = din("w_ge", [128, 8, 36])
    b_ge_rep = din("b_ge_rep", [128, 36])
    moe_w1 = din("moe_w1", [NE, D, DE])
    moe_w3 = din("moe_w3", [NE, D, DE])
    moe_w2 = din("moe_w2", [NE, DE, D])
    fg_rep = din("fg_rep", [128, D])
    out = nc.dram_tensor("out", [S, D], F32, kind="ExternalOutput").ap()

    u1_s = dscr("u1_s", [128, 8, S], BF16)
    qlat_s = dscr("qlat_s", [128, 2, NH, S], BF16)
    qi_s = dscr("qi_s", [128, 4, S], BF16)
    sg_s = dscr("sg_s", [128, 16, S], BF16)
    ya_s = dscr("ya_s", [128, 8, S], BF16)
    att_s = dscr("att_s", [128, 8, S], BF16)
    h_s = dscr("h_s", [S, D], F32)
    u2_s = dscr("u2_s", [128, 8, S], BF16)
    dbg = {}
    if debug:
        dbg["score"] = nc.dram_tensor("dbg_score", [NB, 128, S], F32, kind="ExternalOutput").ap()
        dbg["thr"] = nc.dram_tensor("dbg_thr", [128, NB, 4], F32, kind="ExternalOutput").ap()
        dbg["wt"] = nc.dram_tensor("dbg_wt", [128, NB, NE], F32, kind="ExternalOutput").ap()

    SC = Sched(nc)
    op = SC.op
    V, A, P, T = nc.vector, nc.scalar, nc.gpsimd, nc.tensor

    def MM(o, terms, reads, writes):
        def f():
            n = len(terms)
            ins = None
            for i, (l, r) in enumerate(terms):
                ins = T.matmul(o, lhsT=l, rhs=r, start=(i == 0), stop=(i == n - 1))
            return ins
        return op("pe", f, reads, writes)

    def wload(dst, src, q="pool"):
        n = dst.shape[1]
        for i in range(n):
            SC.dma(q, dst[:, i], src[:, i], writes=[dst])

    uid = [0]

    def sbt(es, name, shape, dt):
        uid[0] += 1
        return es.enter_context(nc.sbuf_tensor(f"{name}_{uid[0]}", list(shape), dt))

    def pst(es, name, shape, dt=F32):
        uid[0] += 1
        return es.enter_context(nc.psum_tensor(f"{name}_{uid[0]}", list(shape), dt))

    def mod_rows(grow, g0, pr):
        with ExitStack() as es:
            cc = sbt(es, "ccr", [128, 8], F32)
            scl = sbt(es, "sclr", [128, 8], F32)
            screp = sbt(es, "screp", [128, 8, 128], F32)
            wm = sbt(es, "wmr", [128, 8, 512], F32)
            brow = sbt(es, "brow", [128, 512], F32)
            SC.dma("sp", cc[:], c_col, writes=[cc])
            op("act", lambda: A.activation(out=scl[:], in_=cc[:], func=AF.Silu), reads=[cc], writes=[scl])
            for kc in range(8):
                op("dve", lambda kc=kc: V.tensor_copy(out=screp[:, kc, :], in_=scl[:, kc:kc + 1].to_broadcast([128, 128])),
                   reads=[scl], writes=[screp])
            for half in range(2):
                g = g0 + half
                SC.dma("sp", wm[:], w_mod[:, :, g * 512:(g + 1) * 512], writes=[wm])
                SC.dma("sp", brow[:], b_mod_rep[:, g * 512:(g + 1) * 512], writes=[brow])
                MM(pr[:], [(screp[:, kc, :], wm[:, kc, :]) for kc in range(8)], [screp, wm], [pr])
                op("dve", lambda half=half: V.tensor_tensor(out=grow[:, half * 512:(half + 1) * 512], in0=pr[:],
                                                            in1=brow[:], op=ALU.add), reads=[pr, brow], writes=[grow])
            SC.barrier()

    with ExitStack() as es0:
        eps_c = sbt(es0, "eps_c", [128, 1], F32)
        one_c = sbt(es0, "one_c", [128, 1], F32)
        ident_f = sbt(es0, "ident_f", [128, 128], F32)
        ident_b = sbt(es0, "ident_b", [128, 128], BF16)
        ones_b = sbt(es0, "ones_b", [128, 128], BF16)
        irep = sbt(es0, "irep", [128, 4, 128], BF16)
        caus = sbt(es0, "caus", [128, 128], F32)
        cpos = sbt(es0, "cpos", [128, 128], F32)
        gpos = sbt(es0, "gpos", [128, 128], F32)
        nsl = sbt(es0, "nsl", [128, NH], F32)
        A1 = sbt(es0, "A1", [128, 8], F32)
        B1 = sbt(es0, "B1", [128, 8], F32)
        A2 = sbt(es0, "A2", [128, 8], F32)
        B2 = sbt(es0, "B2", [128, 8], F32)

        op("pool", lambda: P.memset(eps_c[:], EPS), writes=[eps_c])
        op("pool", lambda: P.memset(one_c[:], 1.0), writes=[one_c])
        op("pool", lambda: P.memset(ident_f[:], 1.0), writes=[ident_f])
        op("pool", lambda: P.affine_select(out=ident_f[:], in_=ident_f[:], pattern=[[-1, 128]],
                                           compare_op=ALU.is_equal, fill=0.0, base=0, channel_multiplier=1),
           reads=[ident_f], writes=[ident_f])
        op("dve", lambda: V.tensor_copy(out=ident_b[:], in_=ident_f[:]), reads=[ident_f], writes=[ident_b])
        op("pool", lambda: P.memset(ones_b[:], 1.0), writes=[ones_b])
        for r_ in range(4):
            op("dve", lambda r_=r_: V.tensor_copy(out=irep[:, r_, :], in_=ident_f[:]), reads=[ident_f], writes=[irep])
        op("pool", lambda: P.memset(caus[:], 0.0), writes=[caus])
        op("pool", lambda: P.affine_select(out=caus[:], in_=caus[:], pattern=[[-1, 128]],
                                           compare_op=ALU.is_ge, fill=NEG, base=0, channel_multiplier=1),
           reads=[caus], writes=[caus])
        op("dve", lambda: V.tensor_scalar(out=cpos[:], in0=caus[:], scalar1=-1.0, scalar2=None, op0=ALU.mult),
           reads=[caus], writes=[cpos])
        op("pool", lambda: P.iota(gpos[:], pattern=[[64, 128]], base=63, channel_multiplier=0,
                                  allow_small_or_imprecise_dtypes=True), writes=[gpos])
        for h in range(NH):
            op("pool", lambda h=h: P.memset(nsl[:, h:h + 1], -(2.0 ** -(h + 1))), writes=[nsl])

        with ExitStack() as es:
            cc = sbt(es, "cc", [128, 8], F32)
            scl = sbt(es, "scl", [128, 8], F32)
            bmc = sbt(es, "bmc", [128, 48], F32)
            modT = sbt(es, "modT", [128, 48], F32)
            n1c = sbt(es, "n1c", [128, 8], F32)
            n2c = sbt(es, "n2c", [128, 8], F32)
            wm = [sbt(es, f"wm{i}", [128, 8, 512], F32) for i in range(2)]
            pm = pst(es, "pm", [128, 48])
            SC.dma("sp", cc[:], c_col, writes=[cc])
            SC.dma("sp", bmc[:], b_mod_col, writes=[bmc])
            SC.dma("sp", n1c[:], n1_col, writes=[n1c])
            SC.dma("sp", n2c[:], n2_col, writes=[n2c])
            op("act", lambda: A.activation(out=scl[:], in_=cc[:], func=AF.Silu), reads=[cc], writes=[scl])
            for g in range(12):
                wb = wm[g % 2]
                SC.dma("sp", wb[:], w_mod[:, :, g * 512:(g + 1) * 512], writes=[wb])
                def f(g=g, wb=wb):
                    ins = None
                    for j in range(4):
                        for kc in range(8):
                            ins = T.matmul(pm[:, 4 * g + j:4 * g + j + 1], lhsT=wb[:, kc, j * 128:(j + 1) * 128],
                                           rhs=scl[:, kc:kc + 1], start=(kc == 0), stop=(kc == 7))
                    return ins
                op("pe", f, reads=[wb, scl], writes=[pm])
            op("dve", lambda: V.tensor_tensor(out=modT[:], in0=pm[:], in1=bmc[:], op=ALU.add),
               reads=[pm, bmc], writes=[modT])
            op("dve", lambda: V.scalar_tensor_tensor(out=A1[:], in0=modT[:, 8:16], scalar=1.0, in1=n1c[:],
                                                     op0=ALU.add, op1=ALU.mult), reads=[modT, n1c], writes=[A1])
            op("dve", lambda: V.tensor_copy(out=B1[:], in_=modT[:, 0:8]), reads=[modT], writes=[B1])
            op("dve", lambda: V.scalar_tensor_tensor(out=A2[:], in0=modT[:, 32:40], scalar=1.0, in1=n2c[:],
                                                     op0=ALU.add, op1=ALU.mult), reads=[modT, n2c], writes=[A2])
            op("dve", lambda: V.tensor_copy(out=B2[:], in_=modT[:, 24:32]), reads=[modT], writes=[B2])
            SC.barrier()

        with ExitStack() as es:
            wf = sbt(es, "wf1", [128, 8, FM1], BF16)
            wrn = sbt(es, "wrn", [128, NRB, D], BF16)
            wa = sbt(es, "wa", [128, NRB, 128], BF16)
            wx = sbt(es, "wx", [128, NRB, 128], BF16)
            cw = sbt(es, "cw", [128, NRB, 4], F32)
            cb = sbt(es, "cb", [128, NRB], F32)
            ba = sbt(es, "ba", [128, NRB], F32)
            bx = sbt(es, "bx", [128, NRB], F32)
            lm = sbt(es, "lm", [128, NRB], F32)
            cL = sbt(es, "cL", [128, NRB], F32)
            cL2 = sbt(es, "cL2", [128, NRB], F32)
            hprev = sbt(es, "hprev", [128, NRB], F32)
            xr_buf = sbt(es, "xr_buf", [128, NRB, 515], F32)
            xs = [sbt(es, f"xs{i}", [128, D], F32) for i in range(2)]
            xn = [sbt(es, f"xn{i}", [128, D], BF16) for i in range(2)]
            junk = sbt(es, "junkA", [128, D], BF16)
            ssq = [sbt(es, f"ssq{i}", [128, 1], F32) for i in range(2)]
            rs = [sbt(es, f"rs{i}", [128, 1], F32) for i in range(2)]
            uT = sbt(es, "uT", [128, 8, 512], BF16)
            gg = sbt(es, "gg", [128, NRB, 512], BF16)
            hg = gg
            sgb = [sbt(es, f"sgb{i}", [128, 512], BF16) for i in range(2)] * 2
            yab = [sbt(es, f"yab{i}", [128, 512], BF16) for i in range(2)]
            x2 = [sbt(es, f"x2_{i}", [128, 512], F32) for i in range(1)] * 2
            tt = [sbt(es, f"tt_{i}", [128, 512], F32) for i in range(1)] * 2
            xc = [sbt(es, f"xc{i}", [128, 512], F32) for i in range(2)]
            xcb = [sbt(es, f"xcb{i}", [128, 512], BF16) for i in range(2)]
            rr = [sbt(es, f"rr{i}", [128, 512], F32) for i in range(2)]
            ig = [sbt(es, f"ig{i}", [128, 512], F32) for i in range(2)]
            aa = [sbt(es, f"aa{i}", [128, 512], F32) for i in range(2)]
            a2 = [sbt(es, f"a2{i}", [128, 512], F32) for i in range(2)]
            bb = [sbt(es, f"bb{i}", [128, 512], F32) for i in range(2)]
            hs = [sbt(es, f"hs{i}", [128, 512], F32) for i in range(2)]
            ptb = pst(es, "ptbA", [128, 8, 128], BF16)
            pf = [pst(es, f"pfA{i}", [128, 512]) for i in range(2)]
            prga = [pst(es, f"prgaA{i}", [128, 512]) for i in range(2)]
            prgx = [pst(es, f"prgxA{i}", [128, 512]) for i in range(2)]
            pya = [pst(es, f"pyaA{i}", [128, 512]) for i in range(1)] * 2

            wload(wf, w_fm1)
            wload(wrn, w_rnn)
            SC.dma("pool", wa[:], w_rg_a, writes=[wa])
            SC.dma("pool", wx[:], w_rg_x, writes=[wx])
            for (dst, src) in ((cw, conv_w), (cb, conv_b), (ba, b_rg_a), (bx, b_rg_x), (lm, lam)):
                SC.dma("sp", dst[:], src, writes=[dst])
            op("act", lambda: A.activation(out=cL[:], in_=lm[:], func=AF.Exp, scale=-1.0), reads=[lm], writes=[cL])
            op("act", lambda: A.activation(out=cL[:], in_=cL[:], func=AF.Ln, bias=one_c[:], scale=1.0),
               reads=[cL, one_c], writes=[cL])
            op("dve", lambda: V.tensor_scalar(out=cL2[:], in0=cL[:], scalar1=-16.0, scalar2=None, op0=ALU.mult),
               reads=[cL], writes=[cL2])
            op("dve", lambda: V.tensor_scalar(out=cL[:], in0=cL[:], scalar1=-8.0, scalar2=None, op0=ALU.mult),
               reads=[cL], writes=[cL])
            op("pool", lambda: P.memset(hprev[:], 0.0), writes=[hprev])
            op("pool", lambda: P.memset(xr_buf[:], 0.0), writes=[xr_buf])

            for t in range(NT):
                t0 = t * 512
                for s in range(4):
                    b = s % 2
                    SC.dma("sp", xs[b][:], x[t0 + s * 128:t0 + (s + 1) * 128, :], writes=[xs[b]])
                    op("act", lambda b=b: A.activation(out=junk[:], in_=xs[b][:], func=AF.Square, accum_out=ssq[b][:]),
                       reads=[xs[b]], writes=[junk, ssq[b]])
                    op("act", lambda b=b: A.activation(out=rs[b][:], in_=ssq[b][:], func=AF.Sqrt, bias=eps_c[:],
                                                       scale=1.0 / D), reads=[ssq[b], eps_c], writes=[rs[b]])
                    op("dve", lambda b=b: V.reciprocal(out=rs[b][:], in_=rs[b][:]), reads=[rs[b]], writes=[rs[b]])
                    op("dve", lambda b=b: V.tensor_scalar(out=xn[b][:], in0=xs[b][:], scalar1=rs[b][:], scalar2=None,
                                                          op0=ALU.mult), reads=[xs[b], rs[b]], writes=[xn[b]])
                    def tr(b=b):
                        ins = None
                        for c in range(8):
                            ins = T.transpose(out=ptb[:, c, :], in_=xn[b][:, c * 128:(c + 1) * 128], identity=ident_b[:])
                        return ins
                    op("pe", tr, reads=[xn[b], ident_b], writes=[ptb])
                    for c in range(8):
                        if c % 2 == 0:
                            op("dve", lambda c=c, s=s: V.tensor_scalar(
                                out=uT[:, c, s * 128:(s + 1) * 128], in0=ptb[:, c, :], scalar1=A1[:, c:c + 1],
                                scalar2=B1[:, c:c + 1], op0=ALU.mult, op1=ALU.add), reads=[ptb, A1, B1], writes=[uT])
                        else:
                            op("act", lambda c=c, s=s: A.activation(
                                out=uT[:, c, s * 128:(s + 1) * 128], in_=ptb[:, c, :], func=AF.Identity,
                                bias=B1[:, c:c + 1], scale=A1[:, c:c + 1]), reads=[ptb, A1, B1], writes=[uT])
                SC.dma("sp", u1_s[:, :, t0:t0 + 512], uT[:], reads=[uT])
                op("pool", lambda: P.tensor_copy(out=xr_buf[:, :, 0:3], in_=xr_buf[:, :, 512:515]),
                   reads=[xr_buf], writes=[xr_buf])
                for j in range(FM1 // 128):
                    pp = pf[j % 2]
                    MM(pp[:], [(wf[:, kc, j * 128:(j + 1) * 128], uT[:, kc, :]) for kc in range(8)], [wf, uT], [pp])
                    if j < 10:
                        op("act", lambda pp=pp, j=j: A.copy(out=xr_buf[:, j, 3:515], in_=pp[:]),
                           reads=[pp], writes=[xr_buf])
                    elif j < 20:
                        n = j - 10
                        b = n % 2
                        op("act", lambda pp=pp, b=b: A.activation(out=x2[b][:], in_=pp[:], func=AF.Square),
                           reads=[pp], writes=[x2[b]])
                        op("dve", lambda b=b: V.tensor_scalar(out=x2[b][:], in0=x2[b][:], scalar1=0.044715, scalar2=1.0,
                                                              op0=ALU.mult, op1=ALU.add), reads=[x2[b]], writes=[x2[b]])
                        op("dve", lambda pp=pp, b=b: V.tensor_tensor(out=x2[b][:], in0=x2[b][:], in1=pp[:], op=ALU.mult),
                           reads=[x2[b], pp], writes=[x2[b]])
                        op("act", lambda b=b: A.activation(out=tt[b][:], in_=x2[b][:], func=AF.Sigmoid,
                                                           scale=1.5957691216057308), reads=[x2[b]], writes=[tt[b]])
                        op("dve", lambda pp=pp, b=b, n=n: V.tensor_tensor(out=gg[:, n, :], in0=tt[b][:], in1=pp[:],
                                                                          op=ALU.mult), reads=[tt[b], pp], writes=[gg])
                    else:
                        m = j - 20
                        sb_ = sgb[m % 4]
                        op("act", lambda pp=pp, sb_=sb_: A.activation(out=sb_[:], in_=pp[:], func=AF.Sigmoid),
                           reads=[pp], writes=[sb_])
                        SC.dma("sp", sg_s[:, m, t0:t0 + 512], sb_[:], reads=[sb_])
                def stage1(n):
                    b = n % 2
                    op("dve", lambda n=n, b=b: V.tensor_scalar(out=xc[b][:], in0=xr_buf[:, n, 0:512],
                                                               scalar1=cw[:, n, 0:1], scalar2=cb[:, n:n + 1],
                                                               op0=ALU.mult, op1=ALU.add),
                       reads=[xr_buf, cw, cb], writes=[xc[b]])
                    for k in range(1, 4):
                        op("dve", lambda n=n, b=b, k=k: V.scalar_tensor_tensor(
                            out=xc[b][:], in0=xr_buf[:, n, k:k + 512], scalar=cw[:, n, k:k + 1], in1=xc[b][:],
                            op0=ALU.mult, op1=ALU.add), reads=[xr_buf, cw, xc[b]], writes=[xc[b]])
                    op("act", lambda b=b: A.copy(out=xcb[b][:], in_=xc[b][:]), reads=[xc[b]], writes=[xcb[b]])
                    MM(prga[b][:], [(wa[:, n, :], xcb[b][:])], [wa, xcb[b]], [prga[b]])
                    MM(prgx[b][:], [(wx[:, n, :], xcb[b][:])], [wx, xcb[b]], [prgx[b]])

                def stage2(n):
                    b = n % 2
                    op("act", lambda n=n, b=b: A.activation(out=rr[b][:], in_=prga[b][:], func=AF.Sigmoid,
                                                            bias=ba[:, n:n + 1], scale=1.0),
                       reads=[prga[b], ba], writes=[rr[b]])
                    op("act", lambda n=n, b=b: A.activation(out=ig[b][:], in_=prgx[b][:], func=AF.Sigmoid,
                                                            bias=bx[:, n:n + 1], scale=1.0),
                       reads=[prgx[b], bx], writes=[ig[b]])
                    op("act", lambda n=n, b=b: A.activation(out=aa[b][:], in_=rr[b][:], func=AF.Exp,
                                                            scale=cL[:, n:n + 1]), reads=[rr[b], cL], writes=[aa[b]])
                    op("act", lambda n=n, b=b: A.activation(out=a2[b][:], in_=rr[b][:], func=AF.Exp,
                                                            scale=cL2[:, n:n + 1]), reads=[rr[b], cL2], writes=[a2[b]])
                    op("act", lambda b=b: A.activation(out=a2[b][:], in_=a2[b][:], func=AF.Sqrt, bias=one_c[:],
                                                       scale=-1.0), reads=[a2[b], one_c], writes=[a2[b]])
                    op("pool", lambda b=b: P.tensor_tensor(out=bb[b][:], in0=ig[b][:], in1=xc[b][:], op=ALU.mult),
                       reads=[ig[b], xc[b]], writes=[bb[b]])
                    op("dve", lambda b=b: V.tensor_tensor(out=bb[b][:], in0=bb[b][:], in1=a2[b][:], op=ALU.mult),
                       reads=[bb[b], a2[b]], writes=[bb[b]])
                    op("dve", lambda n=n, b=b: V.tensor_tensor_scan(out=hs[b][:], data0=aa[b][:], data1=bb[b][:],
                                                                    initial=hprev[:, n:n + 1], op0=ALU.mult,
                                                                    op1=ALU.add),
                       reads=[aa[b], bb[b], hprev], writes=[hs[b]])
                    op("dve", lambda n=n, b=b: V.tensor_copy(out=hprev[:, n:n + 1], in_=hs[b][:, 511:512]),
                       reads=[hs[b]], writes=[hprev])
                    op("pool", lambda n=n, b=b: P.tensor_tensor(out=hg[:, n, :], in0=hs[b][:], in1=gg[:, n, :],
                                                                op=ALU.mult), reads=[hs[b], gg], writes=[hg])

                stage1(0)
                for n in range(NRB):
                    if n + 1 < NRB:
                        stage1(n + 1)
                    stage2(n)
                pyas = [pya[0], pf[0], pf[1]]
                for dc in range(8):
                    pp = pyas[dc % 3]
                    MM(pp[:], [(wrn[:, n, dc * 128:(dc + 1) * 128], hg[:, n, :]) for n in range(NRB)], [wrn, hg], [pp])
                    yb_ = yab[dc % 2]
                    if dc % 2 == 0:
                        op("act", lambda pp=pp, yb_=yb_: A.copy(out=yb_[:], in_=pp[:]), reads=[pp], writes=[yb_])
                    else:
                        op("dve", lambda pp=pp, yb_=yb_: V.tensor_copy(out=yb_[:], in_=pp[:]), reads=[pp], writes=[yb_])
                    SC.dma("sp", ya_s[:, dc, t0:t0 + 512], yb_[:], reads=[yb_])
            SC.barrier()

        with ExitStack() as esm:
            kiT2 = sbt(esm, "kiT2", [128, S], BF16)
            Cc = sbt(esm, "Cc", [128, NB, R], BF16)
            CT = sbt(esm, "CT", [128, 2, S], BF16)
            WI = sbt(esm, "WI", [128, NB, 8], F32)

            with ExitStack() as es:
                wf = sbt(es, "wf2", [128, 8, FM2], BF16)
                wt = sbt(es, "wtm", [128, 8, TMC], BF16)
                wuk = sbt(es, "wuk", [128, NH, R], BF16)
                gkv = sbt(es, "gkv", [128, R], F32)
                uT2 = [sbt(es, f"uT2_{i}", [128, 8, 512], BF16) for i in range(2)]
                qT = sbt(es, "qT", [128, 8, 512], BF16)
                qlb = [sbt(es, f"qlb{i}", [128, 2, 512], BF16) for i in range(2)]
                qib = sbt(es, "qib", [128, 4, 512], BF16)
                junk = sbt(es, "junkA2", [128, R], F32)
                ssq = [sbt(es, f"ssqc{i}", [128, 1], F32) for i in range(2)]
                rs = [sbt(es, f"rsc{i}", [128, 1], F32) for i in range(2)]
                pf = [pst(es, f"pfB{i}", [128, 512]) for i in range(3)]
                ptm = [pst(es, f"ptm{i}", [128, 512]) for i in range(2)]
                ptb = pst(es, "ptbB", [128, 2, 128], BF16)
                pql = [pst(es, f"pql{i}", [128, 512]) for i in range(2)]
                wload(wf, w_fm2)
                SC.dma("pool", wt[:], w_tm, writes=[wt])
                SC.dma("pool", wuk[:], w_ukT, writes=[wuk])
                SC.dma("sp", gkv[:], gkv_rep, writes=[gkv])
                for t in range(NT):
                    t0 = t * 512
                    u = uT2[t % 2]
                    SC.dma("sp", u[:], u1_s[:, :, t0:t0 + 512], writes=[u])
                    for j in range(FM2 // 128):
                        pp = pf[j % 3]
                        MM(pp[:], [(wf[:, kc, j * 128:(j + 1) * 128], u[:, kc, :]) for kc in range(8)], [wf, u], [pp])
                        if j < 8:
                            e_ = "act" if j % 2 else "dve"
                            if e_ == "act":
                                op("act", lambda pp=pp, j=j: A.copy(out=qT[:, j, :], in_=pp[:]), reads=[pp], writes=[qT])
                            else:
                                op("dve", lambda pp=pp, j=j: V.tensor_copy(out=qT[:, j, :], in_=pp[:]),
                                   reads=[pp], writes=[qT])
                        elif j < 12:
                            op("act", lambda pp=pp, j=j: A.copy(out=qib[:, j - 8, :], in_=pp[:]), reads=[pp], writes=[qib])
                        else:
                            op("dve", lambda pp=pp: V.tensor_copy(out=kiT2[:, t0:t0 + 512], in_=pp[:]),
                               reads=[pp], writes=[kiT2])
                    SC.dma("sp", qi_s[:, :, t0:t0 + 512], qib[:], reads=[qib])
                    for h in range(NH):
                        ql = qlb[h % 2]
                        for rc in range(2):
                            pp = pql[rc]
                            MM(pp[:], [(wuk[:, h, rc * 128:(rc + 1) * 128], qT[:, h, :])], [wuk, qT], [pp])
                            if rc == 0:
                                op("act", lambda pp=pp, ql=ql: A.mul(out=ql[:, 0, :], in_=pp[:], mul=ATT_SCALE),
                                   reads=[pp], writes=[ql])
                            else:
                                op("dve", lambda pp=pp, ql=ql: V.tensor_scalar(out=ql[:, 1, :], in0=pp[:],
                                                                               scalar1=ATT_SCALE, scalar2=None,
                                                                               op0=ALU.mult), reads=[pp], writes=[ql])
                        SC.dma("sp", qlat_s[:, :, h, t0:t0 + 512], ql[:], reads=[ql])
                    for s in range(4):
                        blk = t * 4 + s
                        b = s % 2
                        pp = ptm[b]
                        MM(pp[:, 0:TMC], [(u[:, kc, s * 128:(s + 1) * 128], wt[:, kc, :]) for kc in range(8)],
                           [u, wt], [pp])
                        op("act", lambda pp=pp, b=b: A.activation(out=junk[:], in_=pp[:, 0:R], func=AF.Square,
                                                                  accum_out=ssq[b][:]), reads=[pp], writes=[junk, ssq[b]])
                        op("act", lambda b=b: A.activation(out=rs[b][:], in_=ssq[b][:], func=AF.Sqrt, bias=eps_c[:],
                                                           scale=1.0 / R), reads=[ssq[b], eps_c], writes=[rs[b]])
                        op("dve", lambda b=b: V.reciprocal(out=rs[b][:], in_=rs[b][:]), reads=[rs[b]], writes=[rs[b]])
                        op("dve", lambda pp=pp, b=b, blk=blk: V.scalar_tensor_tensor(
                            out=Cc[:, blk, :], in0=pp[:, 0:R], scalar=rs[b][:], in1=gkv[:], op0=ALU.mult, op1=ALU.mult),
                           reads=[pp, rs[b], gkv], writes=[Cc])
                        op("act", lambda pp=pp, blk=blk: A.copy(out=WI[:, blk, :], in_=pp[:, R:R + 8]),
                           reads=[pp], writes=[WI])
                        def tr(blk=blk):
                            ins = None
                            for rc in range(2):
                                ins = T.transpose(out=ptb[:, rc, :], in_=Cc[:, blk, rc * 128:(rc + 1) * 128],
                                                  identity=ident_b[:])
                            return ins
                        op("pe", tr, reads=[Cc, ident_b], writes=[ptb])
                        op("act", lambda blk=blk: A.copy(out=CT[:, :, blk * 128:(blk + 1) * 128], in_=ptb[:]),
                           reads=[ptb], writes=[CT])
                SC.barrier()

            with ExitStack() as es:
                LPOS = sbt(es, "LPOS", [128, NB, 128], BF16)
                RB = [sbt(es, f"RB{i}", [128, 2, 512], BF16) for i in range(2)]
                wuv = sbt(es, "wuv", [128, 2, NH * HD], BF16)
                score = sbt(es, "score", [128, S], F32)
                MB = [sbt(es, f"MB{i}", [128, S], BF16) for i in range(2)]
                qiblk = [sbt(es, f"qiblk{i}", [128, 4, 128], BF16) for i in range(2)]
                qlblk = [sbt(es, f"qlblk{i}", [128, 2, NH, 128], BF16) for i in range(1)] * 2
                Dh = [sbt(es, f"Dh{i}", [128, IH, 128], BF16) for i in range(1)] * 2
                Rh = [sbt(es, f"Rh{i}", [128, 512], BF16) for i in range(4)]
                qz = [sbt(es, f"qz{i}", [128, IH, 128], BF16) for i in range(2)]
                PT = [sbt(es, f"PT{i}", [128, 512], BF16) for i in range(3)]
                m8 = sbt(es, "m8", [128, 8], F32)
                rmin = sbt(es, "rmin", [128, 2], F32)
                tmpd = sbt(es, "tmpd", [128, 128], F32)
                lo = sbt(es, "lo", [128, 1], F32)
                w0 = sbt(es, "w0", [128, 1], F32)
                Hn = sbt(es, "Hn", [128, NIT + 1], F32)
                NHn = sbt(es, "NHn", [128, NIT + 1], F32)
                P2 = sbt(es, "P2", [128, NIT + 1], F32)
                NP2 = sbt(es, "NP2", [128, NIT + 1], F32)
                mid = sbt(es, "mid", [128, 1], F32)
                cnt = sbt(es, "cnt", [128, 1], F32)
                stp = sbt(es, "stp", [128, 1], F32)
                gm = sbt(es, "gm", [128, 128], F32)
                pmx = sbt(es, "pmx", [128, 8], F32)
                pmB = sbt(es, "pmB", [128, 128], F32)
                basef = sbt(es, "basef", [128, NH, 128], F32)
                bhi = sbt(es, "bhi", [128, NH, 128], BF16)
                rcp = sbt(es, "rcp", [128, 512], F32)
                oT = sbt(es, "oT", [128, 2, 512], BF16)
                attb = sbt(es, "attb", [128, NH, 128], BF16)
                dbt = sbt(es, "dbt", [128, 4], F32)
                px = [pst(es, f"px{i}", [128, 512]) for i in range(2)]
                psc = pst(es, "psc", [128, 512])
                pl = [pst(es, f"pl{i}", [128, 512]) for i in range(2)]
                po = [pst(es, f"po{i}", [128, 512]) for i in range(2)]
                prs = pst(es, "prs", [128, 512])

                SC.dma("pool", wuv[:], w_uv, writes=[wuv])
                for z_ in qz:
                    op("pool", lambda z_=z_: P.memset(z_[:], 0.0), writes=[z_])
                for n_ in range(NIT):
                    op("pool", lambda n_=n_: P.memset(P2[:, n_:n_ + 1], 2.0 ** -(n_ + 1)), writes=[P2])
                    e_ = n_ + 2 if n_ < NIT - 1 else n_ + 1
                    op("pool", lambda n_=n_, e_=e_: P.memset(NP2[:, n_:n_ + 1], -(2.0 ** -e_)), writes=[NP2])
                op("pool", lambda: P.memset(LPOS[:], 0.0), writes=[LPOS])
                op("pool", lambda: P.memset(LPOS[0:1, :, :], 1.0), reads=[LPOS], writes=[LPOS])
                op("pool", lambda: P.memset(LPOS[32:33, :, :], 1.0), reads=[LPOS], writes=[LPOS])
                op("pool", lambda: P.iota(LPOS[64:65, :, :], pattern=[[0, NB], [1, 128]], base=0, channel_multiplier=0,
                                          allow_small_or_imprecise_dtypes=True), reads=[LPOS], writes=[LPOS])
                op("pool", lambda: P.iota(LPOS[96:97, :, :], pattern=[[1, NB], [0, 128]], base=0, channel_multiplier=0,
                                          allow_small_or_imprecise_dtypes=True), reads=[LPOS], writes=[LPOS])
                for rb in RB:
                    op("pool", lambda rb=rb: P.memset(rb[:], 0.0), writes=[rb])
                    for hh in range(2):
                        for hl in range(4):
                            sl = 2.0 ** -(hh * 4 + hl + 1)
                            op("pool", lambda rb=rb, hh=hh, hl=hl, sl=sl: P.memset(
                                rb[64:65, hh, hl * 128:(hl + 1) * 128], sl), reads=[rb], writes=[rb])
                            op("pool", lambda rb=rb, hh=hh, hl=hl, sl=sl: P.memset(
                                rb[96:97, hh, hl * 128:(hl + 1) * 128], 128.0 * sl), reads=[rb], writes=[rb])

                def index_phase(i, part):
                    b = i % 2
                    L = (i + 1) * 128
                    q0 = i * 128
                    junkb = MB[b]
                    if part == "A":
                        index_A(i, b, L, q0)
                        bisect(L, junkb, 0, NIT // 2)
                    elif part == "B":
                        bisect(L, junkb, NIT // 2, NIT)
                        index_B(i, b, L)
                    else:
                        index_C(i, b, L)

                def bisect(L, junkb, n0, n1):
                    for n_ in range(n0, n1):
                        op("dve", lambda: V.tensor_scalar(out=junkb[:, 0:L], in0=score[:, 0:L], scalar1=mid[:],
                                                          scalar2=0.0, op0=ALU.is_ge, op1=ALU.add, accum_out=cnt[:]),
                           reads=[score, mid], writes=[junkb, cnt])
                        op("dve", lambda n_=n_: V.tensor_scalar(out=stp[:], in0=cnt[:], scalar1=TOPK - 0.5,
                                                                scalar2=Hn[:, n_:n_ + 1], op0=ALU.is_ge, op1=ALU.mult),
                           reads=[cnt, Hn], writes=[stp])
                        op("dve", lambda n_=n_: V.scalar_tensor_tensor(out=mid[:], in0=stp[:], scalar=NHn[:, n_:n_ + 1],
                                                                       in1=mid[:], op0=ALU.add, op1=ALU.add),
                           reads=[stp, NHn, mid], writes=[mid])

                def index_A(i, b, L, q0):
                    SC.dma("sp", qiblk[b][:], qi_s[:, :, q0:q0 + 128], writes=[qiblk[b]])
                    op("dve", lambda: V.scalar_tensor_tensor(
                        out=Dh[b][:], in0=ident_f[:].unsqueeze(1).to_broadcast([128, IH, 128]), scalar=IDX_SCALE,
                        in1=WI[:, i, :].unsqueeze(2).to_broadcast([128, IH, 128]), op0=ALU.mult, op1=ALU.mult),
                       reads=[ident_f, WI], writes=[Dh[b]])
                    qzv = qz[b][:].rearrange("p (a t) q -> p a t q", t=2)
                    op("pool", lambda: P.tensor_copy(out=qzv[0:64, :, 0, :], in_=qiblk[b][0:64, :, :]),
                       reads=[qiblk[b]], writes=[qz[b]])
                    op("pool", lambda: P.tensor_copy(out=qzv[64:128, :, 1, :], in_=qiblk[b][64:128, :, :]),
                       reads=[qiblk[b]], writes=[qz[b]])
                    nch = (L + 511) // 512
                    steps = [(c, h) for c in range(nch) for h in range(IH)]
                    pxs = [px[0], px[1], pl[0], pl[1]]
                    pscs = [psc, prs]

                    def emit_qk(k):
                        c, h = steps[k]
                        n = min(512, L - c * 512)
                        pp = pxs[k % 4]
                        p0 = (h % 2) * 64
                        MM(pp[:, 0:n], [(qz[b][:, h, :], kiT2[:, c * 512:c * 512 + n])], [qz[b], kiT2], [pp])

                    def emit_relu_acc(k):
                        c, h = steps[k]
                        n = min(512, L - c * 512)
                        pp = pxs[k % 4]
                        rh = Rh[k % 4]
                        pa = pscs[c % 2]
                        if k % 2 == 0:
                            op("act", lambda: A.activation(out=rh[:, 0:n], in_=pp[:, 0:n], func=AF.Relu),
                               reads=[pp], writes=[rh])
                        else:
                            op("dve", lambda: V.tensor_scalar(out=rh[:, 0:n], in0=pp[:, 0:n], scalar1=0.0, scalar2=None,
                                                              op0=ALU.max), reads=[pp], writes=[rh])
                        op("pe", lambda: T.matmul(pa[:, 0:n], lhsT=Dh[b][:, h, :], rhs=rh[:, 0:n], start=(h == 0),
                                                  stop=(h == IH - 1)),
                           reads=[Dh[b], rh] + ([pa] if h == 0 else []), writes=[pa])
                        if h == IH - 1:
                            op("act", lambda: A.copy(out=score[:, c * 512:c * 512 + n], in_=pa[:, 0:n]),
                               reads=[pa], writes=[score])

                    LOOK = 2
                    for k in range(min(LOOK, len(steps))):
                        emit_qk(k)
                    for k in range(len(steps)):
                        if k + LOOK < len(steps):
                            emit_qk(k + LOOK)
                        emit_relu_acc(k)
                    op("dve", lambda: V.tensor_tensor(out=tmpd[:], in0=score[:, L - 128:L], in1=cpos[:], op=ALU.add),
                       reads=[score, cpos], writes=[tmpd])
                    op("dve", lambda: V.tensor_reduce(out=rmin[:, 0:1], in_=tmpd[:], axis=AX.X, op=ALU.min),
                       reads=[tmpd], writes=[rmin])
                    if L > 128:
                        op("dve", lambda: V.tensor_reduce(out=rmin[:, 1:2], in_=score[:, 0:L - 128], axis=AX.X,
                                                          op=ALU.min), reads=[score, rmin], writes=[rmin])
                        op("dve", lambda: V.tensor_tensor(out=lo[:], in0=rmin[:, 0:1], in1=rmin[:, 1:2], op=ALU.min),
                           reads=[rmin], writes=[lo])
                    else:
                        op("dve", lambda: V.tensor_copy(out=lo[:], in_=rmin[:, 0:1]), reads=[rmin], writes=[lo])
                    op("dve", lambda: V.tensor_tensor(out=score[:, L - 128:L], in0=score[:, L - 128:L], in1=caus[:],
                                                      op=ALU.add), reads=[score, caus], writes=[score])
                    op("dve", lambda: V.max(out=m8[:], in_=score[:, 0:L]), reads=[score], writes=[m8])
                    op("dve", lambda: V.tensor_tensor(out=w0[:], in0=m8[:, 0:1], in1=lo[:], op=ALU.subtract),
                       reads=[m8, lo], writes=[w0])
                    op("dve", lambda: V.tensor_scalar(out=Hn[:, 0:NIT], in0=P2[:, 0:NIT], scalar1=w0[:], scalar2=None,
                                                      op0=ALU.mult), reads=[w0, P2], writes=[Hn])
                    op("dve", lambda: V.tensor_scalar(out=NHn[:, 0:NIT], in0=NP2[:, 0:NIT], scalar1=w0[:], scalar2=None,
                                                      op0=ALU.mult), reads=[w0, NP2], writes=[NHn])
                    op("dve", lambda: V.tensor_tensor(out=mid[:], in0=lo[:], in1=Hn[:, 0:1], op=ALU.add),
                       reads=[lo, Hn], writes=[mid])

                def index_B(i, b, L):
                    op("dve", lambda: V.tensor_scalar(out=MB[b][:, 0:L], in0=score[:, 0:L], scalar1=mid[:], scalar2=NEG,
                                                      op0=ALU.is_lt, op1=ALU.mult), reads=[score, mid], writes=[MB[b]])
                    if debug:
                        SC.dma("sp", dbg["score"][i, :, 0:L], score[:, 0:L], reads=[score])
                        op("dve", lambda: V.tensor_copy(out=dbt[:, 0:1], in_=mid[:]), reads=[mid], writes=[dbt])
                        op("dve", lambda: V.tensor_copy(out=dbt[:, 1:2], in_=cnt[:]), reads=[cnt], writes=[dbt])
                    ng = L // 64
                    op("dve", lambda: V.tensor_reduce(out=gm[:, 0:ng],
                                                      in_=MB[b][:, 0:L].rearrange("p (g k) -> p g k", k=64),
                                                      axis=AX.X, op=ALU.max), reads=[MB[b]], writes=[gm])
                    op("dve", lambda: V.scalar_tensor_tensor(out=gm[:, 0:ng], in0=gm[:, 0:ng], scalar=-1.0,
                                                             in1=gpos[:, 0:ng], op0=ALU.is_ge, op1=ALU.mult),
                       reads=[gm, gpos], writes=[gm])
                    op("dve", lambda: V.tensor_reduce(out=pmx[:, 0:1], in_=gm[:, 0:ng], axis=AX.X, op=ALU.max),
                       reads=[gm], writes=[pmx])
                    if debug:
                        op("dve", lambda: V.tensor_copy(out=dbt[:, 2:3], in_=pmx[:, 0:1]), reads=[pmx], writes=[dbt])
                        SC.dma("sp", dbg["thr"][:, i, :], dbt[:], reads=[dbt])
                    op("dve", lambda: V.tensor_copy(out=pmB[:], in_=pmx[:, 0:1].to_broadcast([128, 128])),
                       reads=[pmx], writes=[pmB])

                def index_C(i, b, L):
                    MM(psc[:, 0:128], [(pmB[:], ident_f[:])], [pmB, ident_f], [psc])
                    op("dve", lambda: V.tensor_tensor(
                        out=basef[:], in0=psc[:, 0:128].unsqueeze(1).to_broadcast([128, NH, 128]),
                        in1=nsl[:].unsqueeze(2).to_broadcast([128, NH, 128]), op=ALU.mult),
                       reads=[psc, nsl], writes=[basef])
                    op("dve", lambda: V.tensor_copy(out=bhi[:], in_=basef[:]), reads=[basef], writes=[bhi])
                    op("dve", lambda: V.tensor_tensor(out=basef[:], in0=basef[:], in1=bhi[:], op=ALU.subtract),
                       reads=[basef, bhi], writes=[basef])
                    op("dve", lambda: V.tensor_copy(out=RB[b][0:1, :, :].rearrange("p a (h q) -> p (a h) q", h=4),
                                                    in_=bhi[0:1, :, :]), reads=[bhi], writes=[RB[b]])
                    op("dve", lambda: V.tensor_copy(out=RB[b][32:33, :, :].rearrange("p a (h q) -> p (a h) q", h=4),
                                                    in_=basef[32:33, :, :]), reads=[basef], writes=[RB[b]])

                def attn_phase(i, hsel):
                    b = i % 2
                    q0 = i * 128
                    if hsel == 0:
                        SC.dma("sp", qlblk[b][:], qlat_s[:, :, :, q0:q0 + 128], writes=[qlblk[b]])
                    items = [(hsel, j) for j in range(i + 1)]

                    def emit_L(k):
                        hh, j = items[k]
                        pp = pl[k % 2]
                        terms = [(CT[:, rc, j * 128:(j + 1) * 128],
                                  qlblk[b][:, rc, hh * 4:hh * 4 + 4, :].rearrange("p h q -> p (h q)"))
                                 for rc in range(2)]
                        terms.append((MB[b][:, j * 128:(j + 1) * 128], irep[:].rearrange("p r q -> p (r q)")))
                        terms.append((LPOS[:, j, :], RB[b][:, hh, :]))
                        MM(pp[:], terms, [CT, qlblk[b], MB[b], irep, LPOS, RB[b]], [pp])

                    def emit_rest(k):
                        hh, j = items[k]
                        pp = pl[k % 2]
                        pt = PT[k % 3]
                        op("act", lambda: A.activation(out=pt[:], in_=pp[:], func=AF.Exp), reads=[pp], writes=[pt])

                        def pv():
                            ins = None
                            for rc in range(2):
                                ins = T.matmul(po[rc][:], lhsT=Cc[:, j, rc * 128:(rc + 1) * 128], rhs=pt[:],
                                               start=(j == 0), stop=(j == i))
                            ins = T.matmul(prs[:], lhsT=ones_b[:], rhs=pt[:], start=(j == 0), stop=(j == i))
                            return ins
                        op("pe", pv, reads=[Cc, pt, ones_b] + ([po[0], po[1], prs] if j == 0 else []),
                           writes=[po[0], po[1], prs])
                        if j != i:
                            return
                        op("dve", lambda: V.reciprocal(out=rcp[:], in_=prs[:]), reads=[prs], writes=[rcp])
                        for rc in range(2):
                            op("dve", lambda rc=rc: V.tensor_tensor(out=oT[:, rc, :], in0=po[rc][:], in1=rcp[:],
                                                                    op=ALU.mult), reads=[po[rc], rcp], writes=[oT])
                        pa = px[hh]

                        def av():
                            ins = None
                            for hl in range(4):
                                h = hh * 4 + hl
                                for rc in range(2):
                                    ins = T.matmul(pa[:, hl * 128:(hl + 1) * 128], lhsT=wuv[:, rc, h * 128:(h + 1) * 128],
                                                   rhs=oT[:, rc, hl * 128:(hl + 1) * 128], start=(rc == 0), stop=(rc == 1))
                            return ins
                        op("pe", av, reads=[wuv, oT], writes=[pa])
                        op("act", lambda: A.copy(out=attb[:, hh * 4:hh * 4 + 4, :].rearrange("p h q -> p (h q)"),
                                                 in_=pa[:]), reads=[pa], writes=[attb])

                    emit_L(0)
                    for k in range(len(items)):
                        if k + 1 < len(items):
                            emit_L(k + 1)
                        emit_rest(k)
                    if hsel == 1:
                        SC.dma("sp", att_s[:, :, q0:q0 + 128], attb[:], reads=[attb])

                for i in range(NB + 1):
                    if i < NB:
                        index_phase(i, "A")
                    if i >= 1:
                        attn_phase(i - 1, 0)
                    if i < NB:
                        index_phase(i, "B")
                    if i >= 1:
                        attn_phase(i - 1, 1)
                    if i < NB:
                        index_phase(i, "C")
                SC.barrier()

        WT = sbt(es0, "WT", [128, NB, NE], F32)
        with ExitStack() as es:
            g1row = sbt(es, "g1row", [128, D], F32)
            wat = sbt(es, "wat", [128, 8, D], BF16)
            wo = sbt(es, "wo", [128, 8, D], BF16)
            wge = sbt(es, "wge", [128, 8, 36], F32)
            bge = sbt(es, "bge", [128, 36], F32)
            attT = [sbt(es, f"attT{i}", [128, 8, 512], BF16) for i in range(2)]
            sgT = [sbt(es, f"sgT{i}", [128, 16, 512], BF16) for i in range(2)]
            yaT = [sbt(es, f"yaT{i}", [128, 8, 512], BF16) for i in range(2)]
            t1 = [sbt(es, f"t1_{i}", [128, 512], F32) for i in range(2)]
            t2 = [sbt(es, f"t2_{i}", [128, 512], F32) for i in range(2)]
            mixin = sbt(es, "mixin", [128, 8, 512], BF16)
            xs = [sbt(es, f"xsB{i}", [128, D], F32) for i in range(2)]
            hb = [sbt(es, f"hb{i}", [128, D], F32) for i in range(2)]
            u2 = [sbt(es, f"u2_{i}", [128, D], F32) for i in range(2)]
            junk = sbt(es, "junkB2", [128, D], F32)
            ssq = [sbt(es, f"ssqB{i}", [128, 1], F32) for i in range(2)]
            rs = [sbt(es, f"rsB{i}", [128, 1], F32) for i in range(2)]
            u2Tf = sbt(es, "u2Tf", [128, 8, 128], F32)
            u2Tb = sbt(es, "u2Tb", [128, 8, 512], BF16)
            lg = sbt(es, "lg", [128, 36], F32)
            em = sbt(es, "em", [128, NE], F32)
            sm = sbt(es, "sm", [128, 16], F32)
            m8 = sbt(es, "m8r", [128, 8], F32)
            wta = sbt(es, "wta", [128, NE], F32)
            wtb = sbt(es, "wtb", [128, NE], F32)
            pyb = [pst(es, f"pyb{i}", [128, 512]) for i in range(2)]
            pmx = pst(es, "pmxo", [128, 2, 512])
            ptf = pst(es, "ptf", [128, 8, 128])
            plg = pst(es, "plg", [128, 512])
            mod_rows(g1row, 4, plg)
            wload(wat, w_att)
            wload(wo, w_o)
            SC.dma("sp", wge[:], w_ge, writes=[wge])
            SC.dma("sp", bge[:], b_ge_rep, writes=[bge])
            for t in range(NT):
                t0 = t * 512
                b = t % 2
                SC.dma("sp", attT[b][:], att_s[:, :, t0:t0 + 512], writes=[attT[b]])
                SC.dma("sp", sgT[b][:], sg_s[:, :, t0:t0 + 512], writes=[sgT[b]])
                SC.dma("sp", yaT[b][:], ya_s[:, :, t0:t0 + 512], writes=[yaT[b]])
                for dc in range(8):
                    pp = pyb[dc % 2]
                    d2 = dc % 2
                    MM(pp[:], [(wat[:, h, dc * 128:(dc + 1) * 128], attT[b][:, h, :]) for h in range(8)],
                       [wat, attT[b]], [pp])
                    op("dve", lambda pp=pp, dc=dc, d2=d2: V.tensor_tensor(out=t2[d2][:], in0=sgT[b][:, 8 + dc, :], in1=pp[:],
                                                                          op=ALU.mult), reads=[sgT[b], pp], writes=[t2[d2]])
                    op("pool", lambda dc=dc, d2=d2: P.tensor_tensor(out=t1[d2][:], in0=sgT[b][:, dc, :], in1=yaT[b][:, dc, :],
                                                                    op=ALU.mult), reads=[sgT[b], yaT[b]], writes=[t1[d2]])
                    op("pool", lambda dc=dc, d2=d2: P.tensor_tensor(out=mixin[:, dc, :], in0=t1[d2][:], in1=t2[d2][:],
                                                                    op=ALU.add), reads=[t1[d2], t2[d2]], writes=[mixin])
                def stageA(s):
                    blk = t * 4 + s
                    sb2 = s % 2
                    r0 = t0 + s * 128
                    hcur = hb[sb2]
                    SC.dma("sp", xs[sb2][:], x[r0:r0 + 128, :], writes=[xs[sb2]])
                    def mo(s=s):
                        ins = None
                        for half in range(2):
                            for kc in range(8):
                                ins = T.matmul(pmx[:, half, :], lhsT=mixin[:, kc, s * 128:(s + 1) * 128],
                                               rhs=wo[:, kc, half * 512:(half + 1) * 512], start=(kc == 0), stop=(kc == 7))
                        return ins
                    op("pe", mo, reads=[mixin, wo], writes=[pmx])
                    hcur = hb[sb2]
                    op("dve", lambda hcur=hcur: V.tensor_tensor(out=hcur[:], in0=pmx[:].rearrange("p a b -> p (a b)"),
                                                                in1=g1row[:], op=ALU.mult), reads=[pmx, g1row], writes=[hcur])
                    op("pool", lambda hcur=hcur, sb2=sb2: P.tensor_tensor(out=hcur[:], in0=hcur[:], in1=xs[sb2][:],
                                                                          op=ALU.add), reads=[hcur, xs[sb2]], writes=[hcur])
                    SC.dma("sp", h_s[r0:r0 + 128, :], hcur[:], reads=[hcur])
                    op("act", lambda hcur=hcur, sb2=sb2: A.activation(out=junk[:], in_=hcur[:], func=AF.Square,
                                                                      accum_out=ssq[sb2][:]),
                       reads=[hcur], writes=[junk, ssq[sb2]])
                    op("act", lambda sb2=sb2: A.activation(out=rs[sb2][:], in_=ssq[sb2][:], func=AF.Sqrt, bias=eps_c[:],
                                                           scale=1.0 / D), reads=[ssq[sb2], eps_c], writes=[rs[sb2]])
                    op("dve", lambda sb2=sb2: V.reciprocal(out=rs[sb2][:], in_=rs[sb2][:]), reads=[rs[sb2]], writes=[rs[sb2]])
                    op("dve", lambda hcur=hcur, sb2=sb2: V.tensor_scalar(out=u2[sb2][:], in0=hcur[:], scalar1=rs[sb2][:],
                                                                         scalar2=None, op0=ALU.mult),
                       reads=[hcur, rs[sb2]], writes=[u2[sb2]])

                def stageB(s):
                    blk = t * 4 + s
                    sb2 = s % 2
                    r0 = t0 + s * 128
                    hcur = hb[sb2]
                    def tr(sb2=sb2):
                        ins = None
                        for c in range(8):
                            ins = T.transpose(out=ptf[:, c, :], in_=u2[sb2][:, c * 128:(c + 1) * 128], identity=ident_f[:])
                        return ins
                    op("pe", tr, reads=[u2[sb2], ident_f], writes=[ptf])
                    for c in range(8):
                        op("act", lambda c=c: A.activation(out=u2Tf[:, c, :], in_=ptf[:, c, :], func=AF.Identity,
                                                           bias=B2[:, c:c + 1], scale=A2[:, c:c + 1]),
                           reads=[ptf, A2, B2], writes=[u2Tf])
                    op("dve", lambda s=s: V.tensor_copy(out=u2Tb[:, :, s * 128:(s + 1) * 128], in_=u2Tf[:]),
                       reads=[u2Tf], writes=[u2Tb])
                    MM(plg[:, 0:36], [(u2Tf[:, kc, :], wge[:, kc, :]) for kc in range(8)], [u2Tf, wge], [plg])
                    op("dve", lambda: V.tensor_tensor(out=lg[:], in0=plg[:, 0:36], in1=bge[:], op=ALU.add),
                       reads=[plg, bge], writes=[lg])
                    op("dve", lambda: V.tensor_reduce(out=sm[:, 0:1], in_=lg[:, 0:NG], axis=AX.X, op=ALU.max),
                       reads=[lg], writes=[sm])
                    op("dve", lambda: V.tensor_scalar(out=sm[:, 1:2], in0=sm[:, 0:1], scalar1=-1.0, scalar2=None,
                                                      op0=ALU.mult), reads=[sm], writes=[sm])
                    op("act", lambda: A.activation(out=sm[:, 4:8], in_=lg[:, 0:NG], func=AF.Exp, bias=sm[:, 1:2],
                                                   scale=1.0, accum_out=sm[:, 2:3]), reads=[lg, sm], writes=[sm])
                    op("dve", lambda: V.reciprocal(out=sm[:, 3:4], in_=sm[:, 2:3]), reads=[sm], writes=[sm])
                    op("dve", lambda: V.tensor_scalar(out=sm[:, 8:12], in0=lg[:, 0:NG], scalar1=sm[:, 0:1], scalar2=-1e9,
                                                      op0=ALU.is_lt, op1=ALU.mult), reads=[lg, sm], writes=[sm])
                    for g in range(NG):
                        op("dve", lambda g=g: V.tensor_scalar(out=em[:, g * EPG:(g + 1) * EPG],
                                                              in0=lg[:, NG + g * EPG:NG + (g + 1) * EPG],
                                                              scalar1=sm[:, 8 + g:9 + g], scalar2=None, op0=ALU.add),
                           reads=[lg, sm], writes=[em])
                    op("dve", lambda: V.max(out=m8[:], in_=em[:]), reads=[em], writes=[m8])
                    op("dve", lambda: V.tensor_tensor(out=sm[:, 12:13], in0=m8[:, 1:2], in1=m8[:, 0:1], op=ALU.subtract),
                       reads=[m8], writes=[sm])
                    op("act", lambda: A.activation(out=sm[:, 13:14], in_=sm[:, 12:13], func=AF.Exp), reads=[sm], writes=[sm])
                    op("dve", lambda: V.tensor_scalar(out=sm[:, 14:15], in0=sm[:, 13:14], scalar1=1.0, scalar2=None,
                                                      op0=ALU.add), reads=[sm], writes=[sm])
                    op("dve", lambda: V.reciprocal(out=sm[:, 14:15], in_=sm[:, 14:15]), reads=[sm], writes=[sm])
                    op("dve", lambda: V.tensor_tensor(out=sm[:, 14:15], in0=sm[:, 14:15], in1=sm[:, 3:4], op=ALU.mult),
                       reads=[sm], writes=[sm])
                    op("dve", lambda: V.tensor_tensor(out=sm[:, 15:16], in0=sm[:, 14:15], in1=sm[:, 13:14], op=ALU.mult),
                       reads=[sm], writes=[sm])
                    op("dve", lambda: V.tensor_scalar(out=wta[:], in0=em[:], scalar1=m8[:, 0:1], scalar2=sm[:, 14:15],
                                                      op0=ALU.is_equal, op1=ALU.mult), reads=[em, m8, sm], writes=[wta])
                    op("dve", lambda: V.tensor_scalar(out=wtb[:], in0=em[:], scalar1=m8[:, 1:2], scalar2=sm[:, 15:16],
                                                      op0=ALU.is_equal, op1=ALU.mult), reads=[em, m8, sm], writes=[wtb])
                    op("dve", lambda blk=blk: V.tensor_tensor(out=WT[:, blk, :], in0=wta[:], in1=wtb[:], op=ALU.add),
                       reads=[wta, wtb], writes=[WT])

                stageA(0)
                for s in range(4):
                    if s + 1 < 4:
                        stageA(s + 1)
                    stageB(s)
                SC.dma("sp", u2_s[:, :, t0:t0 + 512], u2Tb[:], reads=[u2Tb])
            if debug:
                SC.dma("sp", dbg["wt"], WT[:], reads=[WT])
            SC.barrier()

        with ExitStack() as es:
            NSB = TC // 128
            NHF = TC // 512
            g2row = sbt(es, "g2row", [128, D], F32)
            ph1 = [pst(es, f"ph1_{i}", [128, 512]) for i in range(2)]
            mod_rows(g2row, 10, ph1[0])
            w1b = [sbt(es, f"w1b{i}", [128, 8, DE], BF16) for i in range(2)]
            w3b = [sbt(es, f"w3b{i}", [128, 8, DE], BF16) for i in range(2)]
            w2b = [sbt(es, f"w2b{i}", [128, 4, D], BF16) for i in range(2)]
            u2Ts = [sbt(es, f"u2T{i}", [128, 8, TC], BF16) for i in range(2)]
            accs = [sbt(es, f"acc{i}", [128, NSB, D], F32) for i in range(2)]
            s1 = [sbt(es, f"s1_{i}", [128, 512], F32) for i in range(2)]
            hid = [sbt(es, f"hid{i}", [128, 4, 512], BF16) for i in range(2)]
            hh_ = [sbt(es, f"hC{i}", [128, D], F32) for i in range(2)]
            junk = sbt(es, "junkC", [128, D], F32)
            ssq = [sbt(es, f"ssqC{i}", [128, 1], F32) for i in range(2)]
            rs = [sbt(es, f"rsC{i}", [128, 1], F32) for i in range(2)]
            fg = sbt(es, "fg", [128, D], F32)
            ph3 = [pst(es, f"ph3_{i}", [128, 512]) for i in range(2)]
            pye = [pst(es, f"pye{i}", [128, 2, 512]) for i in range(2)]
            SC.dma("sp", fg[:], fg_rep, writes=[fg])
            kk = 0
            pending = []
            for tcn in range(NTC):
                c0 = tcn * TC
                u2T = u2Ts[tcn % 2]
                acc = accs[tcn % 2]
                SC.dma("sp", u2T[:], u2_s[:, :, c0:c0 + TC], writes=[u2T])
                for e in range(NE):
                    if pending and e >= 1:
                        pending.pop(0)()
                    wb = e % 2
                    SC.dma("pool", w1b[wb][:], moe_w1[e].rearrange("(kc p) f -> p kc f", p=128), writes=[w1b[wb]])
                    SC.dma("pool", w3b[wb][:], moe_w3[e].rearrange("(kc p) f -> p kc f", p=128), writes=[w3b[wb]])
                    SC.dma("pool", w2b[wb][:], moe_w2[e].rearrange("(fc p) d -> p fc d", p=128), writes=[w2b[wb]])
                    for hf in range(NHF):
                        hd_ = hid[hf % 2]
                        for fc in range(4):
                            p1 = ph1[fc % 2]
                            p3 = ph3[fc % 2]
                            MM(p1[:], [(w1b[wb][:, kc, fc * 128:(fc + 1) * 128], u2T[:, kc, hf * 512:(hf + 1) * 512])
                                       for kc in range(8)], [w1b[wb], u2T], [p1])
                            MM(p3[:], [(w3b[wb][:, kc, fc * 128:(fc + 1) * 128], u2T[:, kc, hf * 512:(hf + 1) * 512])
                                       for kc in range(8)], [w3b[wb], u2T], [p3])
                            sb_ = s1[fc % 2]
                            op("act", lambda p1=p1, sb_=sb_: A.activation(out=sb_[:], in_=p1[:], func=AF.Silu),
                               reads=[p1], writes=[sb_])
                            op("dve", lambda p3=p3, sb_=sb_, hd_=hd_, fc=fc: V.tensor_tensor(
                                out=hd_[:, fc, :], in0=sb_[:], in1=p3[:], op=ALU.mult), reads=[sb_, p3], writes=[hd_])
                        for sub in range(4):
                            pe_ = pye[kk % 2]
                            kk += 1
                            si = hf * 4 + sub
                            blk = (c0 // 128) + si
                            def my(sub=sub, pe_=pe_, hd_=hd_, wb=wb):
                                ins = None
                                for dh in range(2):
                                    for fc in range(4):
                                        ins = T.matmul(pe_[:, dh, :], lhsT=hd_[:, fc, sub * 128:(sub + 1) * 128],
                                                       rhs=w2b[wb][:, fc, dh * 512:(dh + 1) * 512],
                                                       start=(fc == 0), stop=(fc == 3))
                                return ins
                            op("pe", my, reads=[hd_, w2b[wb]], writes=[pe_])
                            if e == 0:
                                op("dve", lambda pe_=pe_, si=si, blk=blk, e=e: V.tensor_scalar(
                                    out=acc[:, si, :], in0=pe_[:].rearrange("p a b -> p (a b)"),
                                    scalar1=WT[:, blk, e:e + 1], scalar2=None, op0=ALU.mult),
                                   reads=[pe_, WT], writes=[acc])
                            else:
                                op("dve", lambda pe_=pe_, si=si, blk=blk, e=e: V.scalar_tensor_tensor(
                                    out=acc[:, si, :], in0=pe_[:].rearrange("p a b -> p (a b)"),
                                    scalar=WT[:, blk, e:e + 1], in1=acc[:, si, :], op0=ALU.mult, op1=ALU.add),
                                   reads=[pe_, WT, acc], writes=[acc])
                def epilogue(si, c0=c0, acc=acc):
                    b = si % 2
                    r0 = c0 + si * 128
                    hc = hh_[b]
                    SC.dma("sp", hc[:], h_s[r0:r0 + 128, :], writes=[hc])
                    op("pool", lambda si=si: P.tensor_tensor(out=acc[:, si, :], in0=acc[:, si, :], in1=g2row[:],
                                                             op=ALU.mult), reads=[acc, g2row], writes=[acc])
                    op("pool", lambda si=si, hc=hc: P.tensor_tensor(out=hc[:], in0=hc[:], in1=acc[:, si, :], op=ALU.add),
                       reads=[hc, acc], writes=[hc])
                    op("act", lambda hc=hc, b=b: A.activation(out=junk[:], in_=hc[:], func=AF.Square,
                                                              accum_out=ssq[b][:]), reads=[hc], writes=[junk, ssq[b]])
                    op("act", lambda b=b: A.activation(out=rs[b][:], in_=ssq[b][:], func=AF.Sqrt, bias=eps_c[:],
                                                       scale=1.0 / D), reads=[ssq[b], eps_c], writes=[rs[b]])
                    op("dve", lambda b=b: V.reciprocal(out=rs[b][:], in_=rs[b][:]), reads=[rs[b]], writes=[rs[b]])
                    op("dve", lambda hc=hc, b=b: V.scalar_tensor_tensor(out=hc[:], in0=hc[:], scalar=rs[b][:], in1=fg[:],
                                                                        op0=ALU.mult, op1=ALU.mult),
                       reads=[hc, rs[b], fg], writes=[hc])
                    SC.dma("sp", out[r0:r0 + 128, :], hc[:], reads=[hc])
                for si in range(NSB):
                    pending.append(lambda si=si, ep=epilogue: ep(si))
            while pending:
                pending.pop(0)()
            SC.barrier()
    SC.close()
    return nc


def _col(v, n):
    return np.ascontiguousarray(np.asarray(v, np.float32).reshape(n, 128).T)


def _kc(w):
    K, N = w.shape
    return np.ascontiguousarray(np.asarray(w, np.float32).reshape(K // 128, 128, N).transpose(1, 0, 2))


def _rep(v):
    v = np.asarray(v, np.float32).reshape(1, -1)
    return np.ascontiguousarray(np.broadcast_to(v, (128, v.shape[1])))


def prep_inputs(inp, b):
    l = 0
    w_in = np.asarray(inp["w_in"][l], np.float32)
    o = np.cumsum([0, DR, DR, NH * HD, R, IH * IDIM, IDIM, IH, 2 * D])
    xr, gr, q, ckv, qi, ki, wi, mg = [w_in[:, o[i]:o[i + 1]] for i in range(8)]
    m = {}
    m["x"] = np.ascontiguousarray(inp["x"][b], dtype=np.float32)
    m["c_col"] = _col(inp["c"][b], 8)
    m["w_mod"] = _kc(inp["w_mod"][l])
    m["b_mod_col"] = _col(inp["b_mod"][l], 48)
    m["b_mod_rep"] = _rep(inp["b_mod"][l])
    m["n1_col"] = _col(inp["norm1_g"][l], 8)
    m["n2_col"] = _col(inp["norm2_g"][l], 8)
    m["w_fm1"] = _kc(np.concatenate([xr, gr, mg], axis=1))
    m["w_fm2"] = _kc(np.concatenate([q, qi, ki, ki], axis=1))
    m["w_tm"] = _kc(np.concatenate([ckv, wi], axis=1))
    m["conv_w"] = np.ascontiguousarray(np.asarray(inp["conv_w"][l], np.float32).reshape(4, NRB, 128).transpose(2, 1, 0))
    m["conv_b"] = _col(inp["conv_b"][l], NRB)
    m["w_rg_a"] = np.ascontiguousarray(np.asarray(inp["w_rg_a"][l], np.float32).transpose(1, 0, 2))
    m["w_rg_x"] = np.ascontiguousarray(np.asarray(inp["w_rg_x"][l], np.float32).transpose(1, 0, 2))
    m["b_rg_a"] = _col(inp["b_rg_a"][l], NRB)
    m["b_rg_x"] = _col(inp["b_rg_x"][l], NRB)
    m["lam"] = _col(inp["lru_lambda"][l], NRB)
    m["gkv_rep"] = _rep(inp["kv_norm_g"][l])
    m["w_ukT"] = np.ascontiguousarray(np.asarray(inp["w_uk"][l], np.float32).transpose(2, 1, 0))
    m["w_uv"] = _kc(np.asarray(inp["w_uv"][l], np.float32).reshape(R, NH * HD))
    m["w_rnn"] = _kc(inp["w_rnn_out"][l])
    m["w_att"] = _kc(inp["w_att_out"][l])
    m["w_o"] = _kc(inp["w_out"][l])
    m["w_ge"] = _kc(np.concatenate([np.asarray(inp["w_group"][l]), np.asarray(inp["w_expert"][l])], axis=1))
    m["b_ge_rep"] = _rep(np.concatenate([np.asarray(inp["b_group"][l]), np.asarray(inp["b_expert"][l])]))
    m["moe_w1"] = np.ascontiguousarray(inp["moe_w1"][l], dtype=np.float32)
    m["moe_w3"] = np.ascontiguousarray(inp["moe_w3"][l], dtype=np.float32)
    m["moe_w2"] = np.ascontiguousarray(inp["moe_w2"][l], dtype=np.float32)
    m["fg_rep"] = _rep(inp["final_g"])
    return m


def kernel(**inputs):
    x = np.asarray(inputs["x"])
    B, S, _ = x.shape
    nc = build(S)
    shared = None
    in_maps = []
    for b in range(B):
        m = prep_inputs(inputs, b)
        if shared is None:
            shared = m
        else:
            for k in m:
                if k not in ("x", "c_col"):
                    m[k] = shared[k]
        in_maps.append(m)
    res = run_bass_kernel_spmd(nc, in_maps, core_ids=list(range(B)))
    return np.stack([np.asarray(r["out"], dtype=np.float32) for r in res.results], axis=0)
```
